# Optimizing a Trainium2 kernel written in Bass

```python
import math
import jax, jax.numpy as jnp
from jax import lax
import numpy as np

D_MODEL = 2048
BATCH = 1
SEQ = 8192
DEPTH = 1

CHUNK = 64
MIX_WIDTH = D_MODEL
MIX_A = MIX_WIDTH // 2
MIX_B = MIX_WIDTH - MIX_A
POOL_WINDOWS = (2, 4, 8, 16)
N_POOL_GROUPS = len(POOL_WINDOWS)
POOL_CH = MIX_A // N_POOL_GROUPS
CONV_HEADS = 8
CONV_HEAD_DIM = MIX_B // CONV_HEADS
CONV_W = 3
IN_COLS = MIX_A + 3 * MIX_B
N_GROUPS = 4
E_PER_GROUP = 8
N_EXPERTS = N_GROUPS * E_PER_GROUP
TOP_K = 2
D_EXPERT = D_MODEL // 2
BLK = 128
EPS = 1e-6

kernel_name = "hybrid_pool_shortconv_hmoe"


def rmsnorm(x, g):
    xf = x.astype(jnp.float32)
    y = xf * lax.rsqrt(jnp.mean(xf * xf, axis=-1, keepdims=True) + EPS)
    return (y * g.astype(jnp.float32)).astype(x.dtype)


def multiscale_pool(u, w_pool, pool_scale):
    bt, s, _ = u.shape
    uf = u.astype(jnp.float32).reshape(bt, s, N_POOL_GROUPS, POOL_CH)
    cs = jnp.cumsum(uf, axis=1)
    steps = jnp.arange(1, s + 1)
    outs = []
    for gi, w in enumerate(POOL_WINDOWS):
        c = cs[:, :, gi]
        lagged = jnp.pad(c, ((0, 0), (w, 0), (0, 0)))[:, :s]
        cnt = jnp.minimum(steps, w).astype(jnp.float32)[None, :, None]
        outs.append((c - lagged) / cnt - uf[:, :, gi])
    pooled = jnp.stack(outs, axis=2).astype(u.dtype)
    mixed = jnp.einsum('bsgc,gcd->bsgd', pooled, w_pool)
    return mixed.reshape(bt, s, MIX_A) * pool_scale


def short_gated_conv(b_gate, c_gate, v, conv_w):
    s = v.shape[1]
    z = c_gate * v
    zp = jnp.pad(z, ((0, 0), (CONV_W - 1, 0), (0, 0)))
    y = zp[:, 0:s] * conv_w[:, 0]
    for k in range(1, CONV_W):
        y = y + zp[:, k:k + s] * conv_w[:, k]
    return b_gate * y


def hierarchical_moe(h, w_rg, b_rg, w_re, b_re, w_gate, w_up, w_down):
    bt, s, d = h.shape
    t = bt * s
    ht = h.reshape(t, d)
    g_prob = jax.nn.softmax((ht @ w_rg).astype(jnp.float32) + b_rg.astype(jnp.float32), axis=-1)
    grp = jnp.argmax(g_prob, axis=-1)
    g_w = jnp.take_along_axis(g_prob, grp[:, None], axis=-1)[:, 0]
    e_logits = ((ht @ w_re).astype(jnp.float32) + b_re.astype(jnp.float32)).reshape(t, N_GROUPS, E_PER_GROUP)
    e_sel = jnp.take_along_axis(e_logits, grp[:, None, None], axis=1)[:, 0]
    e_prob = jax.nn.softmax(e_sel, axis=-1)
    top_p, top_i = lax.top_k(e_prob, TOP_K)
    top_p = top_p / jnp.sum(top_p, axis=-1, keepdims=True)
    weights = g_w[:, None] * top_p
    expert_id = grp[:, None] * E_PER_GROUP + top_i

    a = t * TOP_K
    e_flat = expert_id.reshape(a).astype(jnp.int32)
    w_flat = weights.reshape(a)
    tok_flat = jnp.repeat(jnp.arange(t, dtype=jnp.int32), TOP_K)
    order = jnp.argsort(e_flat, stable=True)
    e_s, tok_s, w_s = e_flat[order], tok_flat[order], w_flat[order]
    counts = jnp.bincount(e_flat, length=N_EXPERTS)
    starts = jnp.cumsum(counts) - counts
    padded = ((counts + BLK - 1) // BLK) * BLK
    pstarts = jnp.cumsum(padded) - padded
    pends = pstarts + padded
    dest = pstarts[e_s] + (jnp.arange(a, dtype=jnp.int32) - starts[e_s])
    n_blocks = (a + N_EXPERTS * (BLK - 1) + BLK - 1) // BLK
    n_pad = n_blocks * BLK
    xbuf = jnp.zeros((n_pad, d), h.dtype).at[dest].set(ht[tok_s])
    blk_start = jnp.arange(n_blocks, dtype=jnp.int32) * BLK
    blk_expert = jnp.clip(jnp.sum(pends[None, :] <= blk_start[:, None], axis=1), 0, N_EXPERTS - 1)

    def expert_block(args):
        xb, e = args
        hid = jax.nn.silu(xb @ w_gate[e]) * (xb @ w_up[e])
        return hid @ w_down[e]

    ybuf = lax.map(expert_block, (xbuf.reshape(n_blocks, BLK, d), blk_expert)).reshape(n_pad, d)
    y_s = ybuf[dest] * w_s[:, None].astype(h.dtype)
    out = jnp.zeros((t, d), h.dtype).at[tok_s].add(y_s)
    return out.reshape(bt, s, d)


def setup_inputs(seed: int = 0) -> dict:
    key = jax.random.key(seed)
    ks = jax.random.split(key, 20)
    f32 = jnp.float32
    nrm = lambda k, shape, scale: jax.random.normal(k, shape, f32) * scale
    return {
        "x": nrm(ks[0], (BATCH, SEQ, D_MODEL), 1.0),
        "norm_mix_g": 1.0 + nrm(ks[1], (DEPTH, D_MODEL), 0.05),
        "w_in": nrm(ks[2], (DEPTH, D_MODEL, IN_COLS), D_MODEL ** -0.5),
        "w_pool": nrm(ks[3], (DEPTH, N_POOL_GROUPS, POOL_CH, POOL_CH), POOL_CH ** -0.5),
        "pool_scale": 1.0 + nrm(ks[4], (DEPTH, MIX_A), 0.1),
        "conv_w": nrm(ks[5], (DEPTH, MIX_B, CONV_W), CONV_W ** -0.5),
        "w_out": nrm(ks[6], (DEPTH, MIX_WIDTH, D_MODEL), MIX_WIDTH ** -0.5),
        "norm_ffn_g": 1.0 + nrm(ks[7], (DEPTH, D_MODEL), 0.05),
        "w_router_group": nrm(ks[8], (DEPTH, D_MODEL, N_GROUPS), D_MODEL ** -0.5),
        "b_router_group": nrm(ks[9], (DEPTH, N_GROUPS), 0.01),
        "w_router_expert": nrm(ks[10], (DEPTH, D_MODEL, N_EXPERTS), D_MODEL ** -0.5),
        "b_router_expert": nrm(ks[11], (DEPTH, N_EXPERTS), 0.01),
        "w_gate": nrm(ks[12], (DEPTH, N_EXPERTS, D_MODEL, D_EXPERT), D_MODEL ** -0.5),
        "w_up": nrm(ks[13], (DEPTH, N_EXPERTS, D_MODEL, D_EXPERT), D_MODEL ** -0.5),
        "w_down": nrm(ks[14], (DEPTH, N_EXPERTS, D_EXPERT, D_MODEL), D_EXPERT ** -0.5),
        "norm_final_g": 1.0 + nrm(ks[15], (D_MODEL,), 0.05),
    }


def reference(x, norm_mix_g, w_in, w_pool, pool_scale, conv_w, w_out, norm_ffn_g,
              w_router_group, b_router_group, w_router_expert, b_router_expert,
              w_gate, w_up, w_down, norm_final_g):
    for l in range(DEPTH):
        h = rmsnorm(x, norm_mix_g[l])
        proj = jnp.einsum('bsd,dc->bsc', h, w_in[l])
        u_pool = proj[..., :MIX_A]
        b_gate = proj[..., MIX_A:MIX_A + MIX_B]
        c_gate = proj[..., MIX_A + MIX_B:MIX_A + 2 * MIX_B]
        v = proj[..., MIX_A + 2 * MIX_B:]
        y_a = multiscale_pool(u_pool, w_pool[l], pool_scale[l])
        y_b = short_gated_conv(b_gate, c_gate, v, conv_w[l])
        mixed = jnp.concatenate([y_a, y_b], axis=-1)
        x = x + jnp.einsum('bsc,cd->bsd', mixed, w_out[l])
        h2 = rmsnorm(x, norm_ffn_g[l])
        x = x + hierarchical_moe(h2, w_router_group[l], b_router_group[l],
                                 w_router_expert[l], b_router_expert[l],
                                 w_gate[l], w_up[l], w_down[l])
    return rmsnorm(x, norm_final_g)
```

```python
import numpy as np
from contextlib import ExitStack
import concourse.bass as bass
import concourse.mybir as mybir
from concourse.bass_utils import run_bass_kernel_spmd

F32 = mybir.dt.float32
BF16 = mybir.dt.bfloat16
AF = mybir.ActivationFunctionType
ALU = mybir.AluOpType
AX = mybir.AxisListType

P = 128
D = 2048
KD = 16
NT = 8
TOK = 1024
HALO = 16
TX = TOK + HALO
NE = 32
DE = 1024
KH = 8
NCORES = 8
EPS = 1e-6
BIG = 1.0e30
NRING = 4
NWIN = 3
MAXFLY = 8


class Tok:
    __slots__ = ("sem", "val", "key")

    def __init__(self, sem, val, key):
        self.sem, self.val, self.key = sem, val, key


class Res:
    __slots__ = ("w", "r", "const")

    def __init__(self, const=False):
        self.w = None
        self.r = []
        self.const = const


class DSem:
    def __init__(self, sem, key):
        self.sem, self.key, self.count = sem, key, 0


class Eng:
    def __init__(self, name, h, sem):
        self.name, self.h, self.sem = name, h, sem
        self.count = 0
        self.waited = {}


class Tracker:
    def __init__(self, nc, es):
        self.nc, self.es = nc, es
        self.engs = {}
        for name, h in (("pe", nc.tensor), ("act", nc.scalar), ("dve", nc.vector),
                        ("pool", nc.gpsimd), ("sp", nc.sync)):
            self.engs[name] = Eng(name, h, es.enter_context(nc.semaphore("e_" + name)))
        self.dsems = []

    def dsem(self, name):
        d = DSem(self.es.enter_context(self.nc.semaphore("d_" + name)), "d_" + name)
        self.dsems.append(d)
        return d

    def _wait(self, E, deps):
        need = {}
        for d in deps:
            if d is None:
                continue
            cur = need.get(d.key)
            if cur is None or cur.val < d.val:
                need[d.key] = d
        for key, d in need.items():
            if E.waited.get(key, 0) < d.val:
                E.h.wait_ge(d.sem, d.val)
                E.waited[key] = d.val

    @staticmethod
    def _deps(R, W, extra):
        deps = list(extra)
        for r in R:
            deps.append(r.w)
        for w in W:
            deps.append(w.w)
            deps.extend(w.r)
        return deps

    @staticmethod
    def _commit(tok, R, W):
        for r in R:
            if not r.const:
                r.r.append(tok)
        for w in W:
            w.w = tok
            w.r = []

    def op(self, eng, emit, R=(), W=(), extra=()):
        E = self.engs[eng]
        self._wait(E, self._deps(R, W, extra))
        ins = emit(E.h)
        E.count += 1
        ins.then_inc(E.sem, 1)
        tok = Tok(E.sem, E.count, E.name)
        self._commit(tok, R, W)
        return tok

    def dma(self, queue, out, in_, ds, R=(), W=(), extra=(), nodep=False):
        E = self.engs[queue]
        if not nodep:
            self._wait(E, self._deps(R, W, extra))
        E.h.dma_start(out=out, in_=in_).then_inc(ds.sem, 16)
        ds.count += 16
        tok = Tok(ds.sem, ds.count, ds.key)
        self._commit(tok, R, W)
        return tok

    def all_tokens(self):
        toks = []
        for E in self.engs.values():
            if E.count > 0:
                toks.append(Tok(E.sem, E.count, E.name))
        for d in self.dsems:
            if d.count > 0:
                toks.append(Tok(d.sem, d.count, d.key))
        return toks

    def barrier(self):
        toks = self.all_tokens()
        for E in self.engs.values():
            self._wait(E, toks)


class Arena:
    def __init__(self, t, nbytes):
        self.t, self.nbytes, self.off = t, nbytes, 0

    def alloc(self, nelem, dtype):
        size = 4 if dtype == F32 else 2
        nb = (nelem * size + 63) // 64 * 64
        off = self.off
        self.off += nb
        assert self.off <= self.nbytes, ("arena overflow", self.off, self.nbytes)
        ap = self.t[:, off // 4:(off + nb) // 4]
        if dtype != F32:
            ap = ap.bitcast(dtype)
        return ap[:, 0:nelem]


ARENA_BYTES = 212480


def build_nc(dbg=False, stop_after=None):
    nc = bass.Bass("TRN2", target_bir_lowering=False)
    dt = nc.dram_tensor
    xh = dt("xh", [TX, D], F32, kind="ExternalInput").ap()
    g1 = dt("g1", [D], F32, kind="ExternalInput").ap()
    g2 = dt("g2", [D], F32, kind="ExternalInput").ap()
    g3 = dt("g3", [D], F32, kind="ExternalInput").ap()
    w_in = dt("w_in", [D, 4096], F32, kind="ExternalInput").ap()
    w_pool = dt("w_pool", [4, 256, 256], F32, kind="ExternalInput").ap()
    w_out = dt("w_out", [D, D], F32, kind="ExternalInput").ap()
    pscale_d = dt("pscale", [P, 8], F32, kind="ExternalInput").ap()
    convw_d = dt("convw", [P, 24], F32, kind="ExternalInput").ap()
    invcnt_d = dt("invcnt", [P, 64], F32, kind="ExternalInput").ap()
    w_r = dt("w_r", [D, 36], F32, kind="ExternalInput").ap()
    b_r = dt("b_r", [P, 36], F32, kind="ExternalInput").ap()
    w_gate = dt("w_gate", [NE, D, DE], F32, kind="ExternalInput").ap()
    w_up = dt("w_up", [NE, D, DE], F32, kind="ExternalInput").ap()
    w_down = dt("w_down", [NE, DE, D], F32, kind="ExternalInput").ap()
    ident_d = dt("ident", [P, P], F32, kind="ExternalInput").ap()
    iota_d = dt("iota", [P, P], F32, kind="ExternalInput").ap()
    tri_d = dt("tri", [P, P], F32, kind="ExternalInput").ap()
    ones_d = dt("ones", [P, P], F32, kind="ExternalInput").ap()
    out = dt("out", [TOK, D], F32, kind="ExternalOutput").ap()
    if dbg:
        dbg_x1 = dt("dbg_x1", [TOK, D], F32, kind="ExternalOutput").ap()
        dbg_rt = dt("dbg_rt", [P, 3 * NT * NE], F32, kind="ExternalOutput").ap()

    with ExitStack() as es:
        arena_t = es.enter_context(nc.sbuf_tensor("arena", [P, ARENA_BYTES // 4], F32))
        banks = [es.enter_context(nc.psum_tensor("pb%d" % i, [P, 512], F32)) for i in range(8)]
        bk = [b[:, :] for b in banks]
        bkb = [b[:, :].bitcast(BF16) for b in banks]
        rb = [Res() for _ in range(8)]
        k = Tracker(nc, es)
        A = Arena(arena_t, ARENA_BYTES)

        xres = A.alloc(NT * D, F32).rearrange("p (i d) -> p i d", i=NT)
        gb = A.alloc(D, F32)
        hT_flat = A.alloc(KD * TX, BF16)
        hT = hT_flat.rearrange("p (k t) -> p k t", k=KD)
        h2 = hT_flat[:, 0:NT * D].rearrange("p (i d) -> p i d", i=NT)
        ident_f = A.alloc(P, F32)
        iota_f = A.alloc(P, F32)
        ident_b = A.alloc(P, BF16)
        tri_b = A.alloc(P, BF16)
        ones_b = A.alloc(P, BF16)
        wr_sb = A.alloc(KD * 36, F32).rearrange("p (k n) -> p k n", k=KD)
        br = A.alloc(36, F32)
        pscale = A.alloc(8, F32)
        convw = A.alloc(24, F32).rearrange("p (j c) -> p j c", j=8)
        invcnt = A.alloc(64, F32).rearrange("p (g t) -> p g t", g=4)
        ss = A.alloc(32, F32)
        rs = A.alloc(32, F32)
        rstd = A.alloc(32, F32)
        Afl = A.alloc(NT * NE, F32).rearrange("p (i e) -> p i e", i=NT)
        Abf = A.alloc(NT * NE, BF16).rearrange("p (i e) -> p i e", i=NT)
        Wb = A.alloc(NT * NE, BF16).rearrange("p (i e) -> p i e", i=NT)
        Wfl = A.alloc(NT * NE, F32).rearrange("p (i e) -> p i e", i=NT)
        rank = A.alloc(NT * NE, F32).rearrange("p (i e) -> p i e", i=NT)
        wslot = A.alloc(NE, F32)
        phase_mark = A.off

        r_cf = Res(const=True)
        r_cb = Res(const=True)
        r_gb = Res()
        r_x = [Res() for _ in range(NT + 1)]
        r_hT = Res()
        r_ss, r_rs, r_rstd = Res(), Res(), Res()

        cd_sp = k.dsem("c_sp")
        cd_pl = k.dsem("c_pl")
        k.dma("sp", gb, g1.partition_broadcast(P), k.dsem("gb"), W=[r_gb], nodep=True)
        gb_ds = k.dsems[-1]

        def load_consts_sp():
            for dst, src in ((ident_f, ident_d[:, :]), (iota_f, iota_d[:, :]), (br, b_r[:, :]),
                             (pscale, pscale_d[:, :]), (convw, convw_d.rearrange("p (j c) -> p j c", j=8)),
                             (invcnt, invcnt_d.rearrange("p (g t) -> p g t", g=4)),
                             (wr_sb, w_r.rearrange("(k p) n -> p k n", p=P))):
                t = k.dma("sp", dst, src, cd_sp, nodep=True)
            r_cf.w = t

        mixT = A.alloc(KD * TOK, BF16).rearrange("p (c t) -> p c t", c=KD)
        win = [A.alloc(KD * P, BF16).rearrange("p (k c) -> p k c", k=KD) for _ in range(NWIN)]
        fb = [A.alloc(TX, F32) for _ in range(4)]
        tA = A.alloc(TX, F32)
        tB = A.alloc(TX, F32)
        hn_off = A.off
        hn = [A.alloc(D, BF16) for _ in range(2)]
        pooledT = [A.alloc(2 * TOK, BF16).rearrange("p (k t) -> p k t", k=2) for _ in range(2)]
        wpool_b = A.alloc(4 * 2 * 256, BF16).rearrange("p (g k d) -> p g k d", g=4, k=2)
        c16 = A.alloc(16, F32)
        xhalo_flat = arena_t[:, (phase_mark + KD * TOK * 2 + NWIN * KD * P * 2) // 4:
                             (phase_mark + KD * TOK * 2 + NWIN * KD * P * 2) // 4 + D]
        xhalo = xhalo_flat

        for dst, src in ((ident_b, ident_d[:, :]), (tri_b, tri_d[:, :]), (ones_b, ones_d[:, :]),
                         (wpool_b, w_pool.rearrange("g (k p) d -> p g k d", p=P))):
            t = k.dma("pool", dst, src, cd_pl, nodep=True)
        r_cb.w = t

        dsx = [k.dsem("x%d" % i) for i in range(NT + 1)]
        xsrc = [None] * (NT + 1)
        npt = [HALO] + [P] * NT
        xtoks = []
        for ti in list(range(1, NT + 1)) + [0]:
            if len(xtoks) >= 2:
                k._wait(k.engs["sp"], [xtoks[-2]])
            if ti == 0:
                xsrc[ti] = xhalo[0:HALO, :]
                xtoks.append(k.dma("sp", xsrc[ti], xh[0:HALO, :], dsx[ti], W=[r_x[ti]], nodep=True))
            else:
                xsrc[ti] = xres[:, ti - 1, :]
                xtoks.append(k.dma("sp", xsrc[ti], xh[HALO + (ti - 1) * P:HALO + ti * P, :], dsx[ti], W=[r_x[ti]],
                                   nodep=True))
        load_consts_sp()

        k.op("dve", lambda h: h.memset(ss[:, :], 1.0), W=[r_ss])
        r_hn = [Res(), Res()]
        r_tA = Res()
        sqjunk = tA[:, 0:1024].bitcast(BF16)

        def norm_stats(ti):
            n = npt[ti]
            k.op("act", lambda h: h.activation(out=sqjunk[0:n, :], in_=xsrc[ti], func=AF.Square,
                                               accum_out=ss[0:n, ti:ti + 1]),
                 R=[r_x[ti]], W=[r_ss, r_tA])
            k.op("act", lambda h: h.activation(out=rs[:, ti:ti + 1], in_=ss[:, ti:ti + 1], func=AF.Sqrt,
                                               scale=1.0 / D, bias=EPS), R=[r_ss], W=[r_rs])
            k.op("dve", lambda h: h.reciprocal(out=rstd[:, ti:ti + 1], in_=rs[:, ti:ti + 1]), R=[r_rs], W=[r_rstd])

        seq = list(range(1, NT + 1)) + [0]
        pos_of = {ti: pos for pos, ti in enumerate(seq)}

        def tp_bank(ti, half):
            return (6 if pos_of[ti] % 2 == 0 else 4) + half

        def norm_apply(ti):
            n = npt[ti]
            par = pos_of[ti] % 2
            hb = hn[par]
            k.op("dve", lambda h: h.scalar_tensor_tensor(
                out=hb[0:n, :], in0=xsrc[ti], scalar=rstd[0:n, ti:ti + 1], in1=gb[0:n, :],
                op0=ALU.mult, op1=ALU.mult), R=[r_x[ti], r_rstd, r_gb], W=[r_hn[par]])
            for half in range(2):
                b = tp_bank(ti, half)

                def emit_tp(h, half=half, b=b):
                    ins = None
                    for j in range(8):
                        c = half * 8 + j
                        ins = h.transpose(bkb[b][:, j * n:(j + 1) * n], hb[0:n, c * P:(c + 1) * P], ident_b[0:n, 0:n])
                    return ins
                k.op("pe", emit_tp, R=[r_hn[par], r_cb], W=[rb[b]])

        def norm_evac(ti):
            n = npt[ti]
            toff = 0 if ti == 0 else HALO + (ti - 1) * P
            for half in range(2):
                b = tp_bank(ti, half)
                dst = hT[:, half * 8:half * 8 + 8, toff:toff + n]
                srcp = bkb[b][:, 0:8 * n].rearrange("p (k n) -> p k n", k=8)
                if half == 0:
                    k.op("act", lambda h, dst=dst, srcp=srcp: h.copy(out=dst, in_=srcp), R=[rb[b]], W=[r_hT])
                else:
                    k.op("dve", lambda h, dst=dst, srcp=srcp: h.tensor_copy(out=dst, in_=srcp), R=[rb[b]], W=[r_hT])

        for ti in seq[0:3]:
            norm_stats(ti)
        for pos, ti in enumerate(seq):
            norm_apply(ti)
            if pos >= 1:
                norm_evac(seq[pos - 1])
            if pos + 3 < len(seq):
                norm_stats(seq[pos + 3])
        norm_evac(seq[-1])
        k.dma("sp", gb, g2.partition_broadcast(P), gb_ds, W=[r_gb])

        order = list(range(8)) + [c for j in range(8) for c in (8 + j, 16 + j, 24 + j)]
        dswin = [k.dsem("win%d" % s) for s in range(NWIN)]
        r_win = [Res() for _ in range(NWIN)]
        r_fb = [Res() for _ in range(4)]
        r_tB, r_c16 = Res(), Res()
        r_pooled = [Res(), Res()]
        r_mix = [Res() for _ in range(KD)]
        pjsets = [(0, 1, 4), (2, 3, 5)]
        pjctr = [0]
        fbctr = [0]

        def win_dma(idx):
            cc = order[idx]
            s = idx % NWIN
            k.dma("pool", win[s], w_in[:, cc * P:(cc + 1) * P].rearrange("(k p) c -> p k c", p=P),
                  dswin[s], W=[r_win[s]])

        for idx in range(NWIN):
            win_dma(idx)

        dswo = [k.dsem("wo%d" % s) for s in range(2)]
        r_wo = [Res(), Res()]
        wout_sl = [arena_t[:, hn_off // 4:hn_off // 4 + 4096].bitcast(BF16).rearrange("p (k c) -> p k c", k=KD),
                   hT_flat[:, 0:8192].rearrange("p (k c) -> p k c", k=KD)]
        wout_extra = [[], []]

        def wout_dma(n):
            s = n % 2
            k.dma("pool", wout_sl[s], w_out[:, n * 512:(n + 1) * 512].rearrange("(k p) c -> p k c", p=P),
                  dswo[s], W=[r_wo[s]], extra=wout_extra[s])

        pending_pool = []

        def flush_pool():
            while pending_pool:
                gi, par = pending_pool.pop(0)
                for m in range(2):
                    pb0, pb1, _ = pjsets[pjctr[0] % 2]
                    pjctr[0] += 1

                    def emit_pm(h, gi=gi, m=m, par=par, pb0=pb0, pb1=pb1):
                        ins = None
                        for tb, pbb in ((0, pb0), (1, pb1)):
                            for kc2 in range(2):
                                ins = h.matmul(bk[pbb], lhsT=wpool_b[:, gi, kc2, m * P:(m + 1) * P],
                                               rhs=pooledT[par][:, kc2, tb * 512:(tb + 1) * 512],
                                               start=(kc2 == 0), stop=(kc2 == 1))
                        return ins
                    tpe = k.op("pe", emit_pm, R=[r_pooled[par], r_cb], W=[rb[pb0], rb[pb1]])
                    mc = 2 * gi + m

                    def emit_pe(h, mc=mc, pb0=pb0, pb1=pb1):
                        h.tensor_scalar(out=mixT[:, mc, 0:512], in0=bk[pb0], scalar1=pscale[:, mc:mc + 1],
                                        scalar2=None, op0=ALU.mult)
                        return h.tensor_scalar(out=mixT[:, mc, 512:1024], in0=bk[pb1], scalar1=pscale[:, mc:mc + 1],
                                               scalar2=None, op0=ALU.mult)
                    tdv = k.op("dve", emit_pe, R=[rb[pb0], rb[pb1], r_cf], W=[r_mix[mc]])
                if gi == 3:
                    wout_extra[0] = [tpe, tdv, r_cb.w]
                    wout_dma(0)

        conv_fb = {}
        for idx, cc in enumerate(order):
            s = idx % NWIN
            b0, b1, bh = pjsets[pjctr[0] % 2]
            pjctr[0] += 1

            def emit_proj(h, s=s, b0=b0, b1=b1, bh=bh):
                ins = None
                for kk in range(KD):
                    st, sp_ = (kk == 0), (kk == KD - 1)
                    h.matmul(bk[b0], lhsT=win[s][:, kk, :], rhs=hT[:, kk, HALO:HALO + 512], start=st, stop=sp_)
                    h.matmul(bk[b1], lhsT=win[s][:, kk, :], rhs=hT[:, kk, HALO + 512:TX], start=st, stop=sp_)
                    ins = h.matmul(bk[bh][:, 0:HALO], lhsT=win[s][:, kk, :], rhs=hT[:, kk, 0:HALO], start=st, stop=sp_)
                return ins
            k.op("pe", emit_proj, R=[r_win[s], r_hT], W=[rb[b0], rb[b1], rb[bh]])
            if idx + NWIN < len(order):
                win_dma(idx + NWIN)
            f = fbctr[0] % 4
            fbctr[0] += 1

            def emit_ev(h, f=f, b0=b0, b1=b1, bh=bh):
                h.copy(out=fb[f][:, HALO:HALO + 512], in_=bk[b0])
                h.copy(out=fb[f][:, HALO + 512:TX], in_=bk[b1])
                return h.copy(out=fb[f][:, 0:HALO], in_=bk[bh][:, 0:HALO])
            k.op("act", emit_ev, R=[rb[b0], rb[b1], rb[bh]], W=[r_fb[f]])
            flush_pool()

            if cc < 8:
                gi, kc = cc // 2, cc % 2
                w = 2 << gi
                par = gi % 2
                u = fb[f]
                cur, rcur, lo, shift = u, r_fb[f], 0, 1
                for l in range(gi + 1):
                    dst, rdst = (tA, r_tA) if l % 2 == 0 else (tB, r_tB)
                    lo2 = lo + shift
                    k.op("dve", lambda h, dst=dst, cur=cur, lo2=lo2, shift=shift: h.tensor_tensor(
                        out=dst[:, lo2:TX], in0=cur[:, lo2:TX], in1=cur[:, lo2 - shift:TX - shift], op=ALU.add),
                        R=[rcur], W=[rdst])
                    cur, rcur, lo, shift = dst, rdst, lo2, shift * 2
                k.op("dve", lambda h, cur=cur, u=u, par=par, kc=kc, w=w: h.scalar_tensor_tensor(
                    out=pooledT[par][:, kc, :], in0=cur[:, HALO:TX], scalar=1.0 / w, in1=u[:, HALO:TX],
                    op0=ALU.mult, op1=ALU.subtract), R=[rcur, r_fb[f]], W=[r_pooled[par]])
                k.op("dve", lambda h, cur=cur, gi=gi: h.tensor_tensor(
                    out=c16[:, :], in0=cur[:, HALO:2 * HALO], in1=invcnt[:, gi, :], op=ALU.mult),
                    R=[rcur, r_cf], W=[r_c16])
                k.op("dve", lambda h, u=u, par=par, kc=kc: h.tensor_tensor(
                    out=pooledT[par][:, kc, 0:HALO], in0=c16[:, :], in1=u[:, HALO:2 * HALO], op=ALU.subtract),
                    R=[r_c16, r_fb[f]], W=[r_pooled[par]])
                if kc == 1:
                    pending_pool.append((gi, par))
            else:
                j = (cc - 8) % 8
                conv_fb.setdefault(j, {})[(cc - 8) // 8] = f
                if cc >= 24:
                    fbb, fc, fv = conv_fb[j][0], conv_fb[j][1], conv_fb[j][2]
                    k.op("dve", lambda h, fc=fc, fv=fv: h.tensor_tensor(
                        out=tA[:, 14:TX], in0=fb[fc][:, 14:TX], in1=fb[fv][:, 14:TX], op=ALU.mult),
                        R=[r_fb[fc], r_fb[fv]], W=[r_tA])
                    k.op("dve", lambda h, j=j: h.tensor_scalar(
                        out=tB[:, HALO:TX], in0=tA[:, 14:TX - 2], scalar1=convw[:, j, 0:1], scalar2=None, op0=ALU.mult),
                        R=[r_tA, r_cf], W=[r_tB])
                    k.op("dve", lambda h, j=j: h.scalar_tensor_tensor(
                        out=tB[:, HALO:TX], in0=tA[:, 15:TX - 1], scalar=convw[:, j, 1:2], in1=tB[:, HALO:TX],
                        op0=ALU.mult, op1=ALU.add), R=[r_tA, r_tB, r_cf], W=[r_tB])
                    k.op("dve", lambda h, j=j: h.scalar_tensor_tensor(
                        out=tB[:, HALO:TX], in0=tA[:, HALO:TX], scalar=convw[:, j, 2:3], in1=tB[:, HALO:TX],
                        op0=ALU.mult, op1=ALU.add), R=[r_tA, r_tB, r_cf], W=[r_tB])
                    k.op("dve", lambda h, j=j, fbb=fbb: h.tensor_tensor(
                        out=mixT[:, 8 + j, :], in0=fb[fbb][:, HALO:TX], in1=tB[:, HALO:TX], op=ALU.mult),
                        R=[r_fb[fbb], r_tB], W=[r_mix[8 + j]])

        hT_users = list(r_hT.r) + [r_hT.w]
        wout_extra[1] = hT_users
        wout_dma(1)
        bctr = 0
        junk1c = fb[0][:, 0:1024].bitcast(BF16)
        for n in range(4):
            s = n % 2
            for i in range(NT):
                b = bctr % 4
                bctr += 1

                def emit_op(h, s=s, i=i, b=b):
                    ins = None
                    for kk in range(KD):
                        ins = h.matmul(bk[b], lhsT=mixT[:, kk, i * P:(i + 1) * P], rhs=wout_sl[s][:, kk, :],
                                       start=(kk == 0), stop=(kk == KD - 1))
                    return ins
                k.op("pe", emit_op, R=[r_wo[s]] + r_mix, W=[rb[b]])
                k.op("dve", lambda h, i=i, n=n, b=b: h.tensor_tensor(
                    out=xres[:, i, n * 512:(n + 1) * 512], in0=bk[b], in1=xres[:, i, n * 512:(n + 1) * 512], op=ALU.add),
                    R=[rb[b], r_x[i + 1]], W=[r_x[i + 1]])
                if n == 3:
                    k.op("act", lambda h, i=i: h.activation(out=junk1c, in_=xres[:, i, :], func=AF.Square,
                                                            accum_out=ss[:, 10 + i:11 + i]),
                         R=[r_x[i + 1]], W=[r_ss, r_fb[0]])
            if n + 2 < 4:
                wout_dma(n + 2)

        if dbg:
            dd = k.dsem("dbg")
            for i in range(NT):
                k.dma("sp", dbg_x1[i * P:(i + 1) * P, :], xres[:, i, :], dd, R=[r_x[i + 1]])

        k.barrier()
        if stop_after == "1C":
            return nc

        ring_off = ARENA_BYTES - NRING * 16384
        ring = [arena_t[:, (ring_off + s * 16384) // 4:(ring_off + (s + 1) * 16384) // 4].bitcast(BF16)
                for s in range(NRING)]
        dsr = [k.dsem("ring%d" % s) for s in range(NRING)]
        r_ring = [Res() for _ in range(NRING)]
        n_exp = NE if stop_after != "E1" else 4
        slabs = []
        for e in range(n_exp):
            for kind in ("g", "u"):
                slabs.append((kind, e, 0))
                slabs.append((kind, e, 1))
            slabs.append(("d", e, 0))
            slabs.append(("d", e, 1))
        slab_ptr = [0]
        slab_toks = []

        def issue_slab():
            if slab_ptr[0] >= len(slabs):
                return
            kind, e, hh = slabs[slab_ptr[0]]
            s = (slab_ptr[0] + 1) % NRING
            slab_ptr[0] += 1
            if kind == "d":
                dst = ring[s].rearrange("p (c n) -> p c n", c=KH)
                src = w_down[e, :, hh * 1024:(hh + 1) * 1024].rearrange("(c p) n -> p c n", p=P)
            else:
                wsrc = w_gate if kind == "g" else w_up
                dst = ring[s].rearrange("p (k n) -> p k n", k=8)
                src = wsrc[e].rearrange("(p k) n -> p k n", p=P)[:, hh * 8:(hh + 1) * 8, :]
            gate = [slab_toks[-MAXFLY]] if len(slab_toks) >= MAXFLY else []
            slab_toks.append(k.dma("pool", dst, src, dsr[s], W=[r_ring[s]], extra=gate))

        for _ in range(NRING - 1):
            issue_slab()

        A.off = phase_mark
        h2f_l = [A.alloc(D, F32) for _ in range(2)]
        h2fT_l = [A.alloc(KD * P, F32).rearrange("p (k t) -> p k t", k=KD) for _ in range(2)]
        junk = A.alloc(D, BF16)
        lg_all = A.alloc(NT * 36, F32).rearrange("p (i n) -> p i n", i=NT)
        r_h2f_l, r_h2fT_l = [Res(), Res()], [Res(), Res()]
        r_h2 = [Res() for _ in range(NT)]
        r_A, r_W, r_rank = Res(), Res(), Res()
        r_lg = Res()

        k.op("act", lambda h: h.activation(out=rs[:, 10:18], in_=ss[:, 10:18], func=AF.Sqrt, scale=1.0 / D, bias=EPS),
             R=[r_ss], W=[r_rs])
        k.op("dve", lambda h: h.reciprocal(out=rstd[:, 10:18], in_=rs[:, 10:18]), R=[r_rs], W=[r_rstd])

        def stage_a1(i):
            h2f, h2fT = h2f_l[i % 2], h2fT_l[i % 2]
            r_h2f, r_h2fT = r_h2f_l[i % 2], r_h2fT_l[i % 2]
            k.op("dve", lambda h: h.scalar_tensor_tensor(
                out=h2f[:, :], in0=xres[:, i, :], scalar=rstd[:, 10 + i:11 + i], in1=gb[:, :],
                op0=ALU.mult, op1=ALU.mult), R=[r_x[i + 1], r_rstd, r_gb], W=[r_h2f])
            k.op("pool", lambda h: h.tensor_copy(out=h2[:, i, :].rearrange("t (k p) -> t k p", p=P),
                                                 in_=h2f[:, :].rearrange("t (p k) -> t k p", k=KD)),
                 R=[r_h2f], W=[r_h2[i]])
            for q in range(4):
                def emit_tf(h, q=q):
                    ins = None
                    for j in range(4):
                        c = 4 * q + j
                        ins = h.transpose(bk[q][:, j * P:(j + 1) * P], h2f[:, c * P:(c + 1) * P], ident_f[:, :])
                    return ins
                k.op("pe", emit_tf, R=[r_h2f, r_cf], W=[rb[q]])
                ev_eng = "act" if q % 2 == 0 else "dve"
                if ev_eng == "act":
                    k.op("act", lambda h, q=q: h.copy(out=h2fT[:, 4 * q:4 * q + 4, :],
                                                      in_=bk[q].rearrange("p (k n) -> p k n", k=4)),
                         R=[rb[q]], W=[r_h2fT])
                else:
                    k.op("dve", lambda h, q=q: h.tensor_copy(out=h2fT[:, 4 * q:4 * q + 4, :],
                                                             in_=bk[q].rearrange("p (k n) -> p k n", k=4)),
                         R=[rb[q]], W=[r_h2fT])

        def stage_a2(i):
            h2fT = h2fT_l[i % 2]
            r_h2fT = r_h2fT_l[i % 2]
            lgb = 4 + 2 * (i % 2)

            def emit_lg(h):
                ins = None
                for kk in range(KD):
                    ins = h.matmul(bk[lgb][:, 0:36], lhsT=h2fT[:, kk, :], rhs=wr_sb[:, kk, :],
                                   start=(kk == 0), stop=(kk == KD - 1))
                return ins
            k.op("pe", emit_lg, R=[r_h2fT, r_cf], W=[rb[lgb]])
            k.op("dve", lambda h: h.tensor_tensor(out=lg_all[:, i, :], in0=bk[lgb][:, 0:36], in1=br[:, :], op=ALU.add),
                 R=[rb[lgb], r_cf], W=[r_lg])

        stage_a1(0)
        for i in range(NT):
            if i + 1 < NT:
                stage_a1(i + 1)
            stage_a2(i)

        def al(n):
            return A.alloc(n, F32)
        gmax, gsum, gw = al(NT), al(NT), al(NT)
        gsh, gexp, gmask, pen = al(NT * 4), al(NT * 4), al(NT * 4), al(NT * 4)
        em, mask1, em2, mask2, w1m, w2m = (al(NT * NE) for _ in range(6))
        top1, top2, dlt, ed, den, p1, wt1, wt2 = (al(NT) for _ in range(8))
        v3 = lambda ap_, n: ap_.rearrange("p (i n) -> p i n", i=NT)
        bc = lambda ap_, n: ap_.unsqueeze(2).to_broadcast([P, NT, n])
        gl = lg_all[:, :, 0:4]
        el = lg_all[:, :, 4:36]
        rr = {n_: Res() for n_ in ("gmax", "gsh", "gexp", "gsum", "gw", "gmask", "pen", "em", "mask1", "em2", "mask2",
                                   "top1", "top2", "dlt", "ed", "den", "p1", "wt1", "wt2", "w1m", "w2m")}
        dv = lambda emit, R, W: k.op("dve", emit, R=R, W=W)
        dv(lambda h: h.reduce_max(out=gmax, in_=gl, axis=AX.X), [r_lg], [rr["gmax"]])
        dv(lambda h: h.tensor_tensor(out=v3(gsh, 4), in0=gl, in1=bc(gmax, 4), op=ALU.subtract), [r_lg, rr["gmax"]], [rr["gsh"]])
        k.op("act", lambda h: h.activation(out=gexp, in_=gsh, func=AF.Exp), R=[rr["gsh"]], W=[rr["gexp"]])
        dv(lambda h: h.reduce_sum(out=gsum, in_=v3(gexp, 4), axis=AX.X), [rr["gexp"]], [rr["gsum"]])
        dv(lambda h: h.reciprocal(out=gw, in_=gsum), [rr["gsum"]], [rr["gw"]])
        dv(lambda h: h.tensor_tensor(out=v3(gmask, 4), in0=gl, in1=bc(gmax, 4), op=ALU.is_equal), [r_lg, rr["gmax"]], [rr["gmask"]])
        dv(lambda h: h.tensor_scalar(out=pen, in0=gmask, scalar1=-1.0, scalar2=BIG, op0=ALU.add, op1=ALU.mult),
           [rr["gmask"]], [rr["pen"]])
        dv(lambda h: h.tensor_tensor(out=em.rearrange("p (i g j) -> p i g j", i=NT, g=4),
                                     in0=el.rearrange("p i (g j) -> p i g j", g=4),
                                     in1=v3(pen, 4).unsqueeze(3).to_broadcast([P, NT, 4, 8]), op=ALU.add),
           [r_lg, rr["pen"]], [rr["em"]])
        dv(lambda h: h.reduce_max(out=top1, in_=v3(em, NE), axis=AX.X), [rr["em"]], [rr["top1"]])
        def per_tile(out_, in_, sc, op):
            def emit(h):
                ins = None
                for i in range(NT):
                    ins = h.tensor_scalar(out=out_[:, i * NE:(i + 1) * NE], in0=in_[:, i * NE:(i + 1) * NE],
                                          scalar1=sc[:, i:i + 1], scalar2=None, op0=op)
                return ins
            return emit
        dv(per_tile(mask1, em, top1, ALU.is_equal), [rr["em"], rr["top1"]], [rr["mask1"]])
        dv(lambda h: h.scalar_tensor_tensor(out=em2, in0=mask1, scalar=-BIG, in1=em, op0=ALU.mult, op1=ALU.add),
           [rr["em"], rr["mask1"]], [rr["em2"]])
        dv(lambda h: h.reduce_max(out=top2, in_=v3(em2, NE), axis=AX.X), [rr["em2"]], [rr["top2"]])
        dv(per_tile(mask2, em2, top2, ALU.is_equal), [rr["em2"], rr["top2"]], [rr["mask2"]])
        dv(lambda h: h.tensor_tensor(out=dlt, in0=top2, in1=top1, op=ALU.subtract), [rr["top1"], rr["top2"]], [rr["dlt"]])
        k.op("act", lambda h: h.activation(out=ed, in_=dlt, func=AF.Exp), R=[rr["dlt"]], W=[rr["ed"]])
        dv(lambda h: h.tensor_scalar(out=den, in0=ed, scalar1=1.0, scalar2=None, op0=ALU.add), [rr["ed"]], [rr["den"]])
        dv(lambda h: h.reciprocal(out=p1, in_=den), [rr["den"]], [rr["p1"]])
        dv(lambda h: h.tensor_tensor(out=wt1, in0=p1, in1=gw, op=ALU.mult), [rr["p1"], rr["gw"]], [rr["wt1"]])
        dv(lambda h: h.tensor_tensor(out=wt2, in0=wt1, in1=ed, op=ALU.mult), [rr["wt1"], rr["ed"]], [rr["wt2"]])
        Afl2 = Afl.rearrange("p i e -> p (i e)")
        Wfl2 = Wfl.rearrange("p i e -> p (i e)")
        dv(lambda h: h.tensor_tensor(out=Afl2, in0=mask1, in1=mask2, op=ALU.add), [rr["mask1"], rr["mask2"]], [r_A])
        dv(lambda h: h.tensor_copy(out=Abf.rearrange("p i e -> p (i e)"), in_=Afl2), [r_A], [r_A])
        dv(per_tile(w1m, mask1, wt1, ALU.mult), [rr["mask1"], rr["wt1"]], [rr["w1m"]])
        dv(per_tile(w2m, mask2, wt2, ALU.mult), [rr["mask2"], rr["wt2"]], [rr["w2m"]])
        dv(lambda h: h.tensor_tensor(out=Wfl2, in0=w1m, in1=w2m, op=ALU.add), [rr["w1m"], rr["w2m"]], [r_W])
        dv(lambda h: h.tensor_copy(out=Wb.rearrange("p i e -> p (i e)"), in_=Wfl2), [r_W], [r_W])

        for i in range(NT):
            def emit_rk(h, i=i):
                ins = None
                for i2 in range(i):
                    h.matmul(bk[5][:, 0:NE], lhsT=ones_b[:, :], rhs=Abf[:, i2, :], start=(i2 == 0), stop=False)
                ins = h.matmul(bk[5][:, 0:NE], lhsT=tri_b[:, :], rhs=Abf[:, i, :], start=(i == 0), stop=True)
                return ins
            k.op("pe", emit_rk, R=[r_A, r_cb], W=[rb[5]])
            k.op("dve", lambda h, i=i: h.tensor_copy(out=rank[:, i, :], in_=bk[5][:, 0:NE]), R=[rb[5]], W=[r_rank])


        if dbg:
            dd2 = k.dsem("dbg2")
            k.dma("sp", dbg_rt[:, 0:NT * NE], Afl.rearrange("p i e -> p (i e)"), dd2, R=[r_A])
            k.dma("sp", dbg_rt[:, NT * NE:2 * NT * NE], Wfl.rearrange("p i e -> p (i e)"), dd2, R=[r_W])
            k.dma("sp", dbg_rt[:, 2 * NT * NE:3 * NT * NE], rank.rearrange("p i e -> p (i e)"), dd2, R=[r_rank])

        assert A.off <= ring_off + 16384, ("router-phase scratch runs into prefetched ring slots", A.off, ring_off)
        k.barrier()

        A.off = phase_mark
        gb_bf = gb.bitcast(BF16)
        Yb0 = gb_bf[:, 0:D]
        Yb = [Yb0, Yb0]
        ST = [gb_bf[:, D + j * TOK:D + (j + 1) * TOK] for j in range(2)]
        ob_off = A.off
        XeT1 = A.alloc(KD * P, BF16).rearrange("p (k s) -> p k s", k=KD)
        XeT = [XeT1, XeT1]
        S0 = A.alloc(NT * P, BF16).rearrange("p (i s) -> p i s", i=NT)
        S = [S0, S0]
        hid = A.alloc(DE, BF16)
        hidT = A.alloc(KH * P, BF16).rearrange("p (c s) -> p c s", c=KH)
        sg = [wr_sb.rearrange("p k n -> p (k n)")[:, 0:512], A.alloc(512, F32)]
        ob_end = A.off
        assert A.off <= ring_off, ("expert-phase buffers run into the weight ring", A.off, ring_off)
        r_Yb0 = Res()
        r_Yb = [r_Yb0, r_Yb0]
        r_ST = [Res(), Res()]
        r_XeT1 = Res()
        r_XeT = [r_XeT1, r_XeT1]
        r_S0 = Res()
        r_S = [r_S0, r_S0]
        r_hid, r_hidT = Res(), Res()
        r_sg = [Res(), Res()]
        r_ws = Res()

        issue_slab()
        use_ptr = [1]

        def next_slab():
            s = use_ptr[0] % NRING
            use_ptr[0] += 1
            return s

        rot = [0]

        def next_bank():
            b = 4 + rot[0] % 2
            rot[0] += 1
            return b

        def combine_groups(e, lo, hi):
            eb = e % 2
            for gidx in range(lo, hi):
                i, nb = gidx // 4, gidx % 4
                b = next_bank()
                k.op("pe", lambda h, i=i, nb=nb, b=b: h.matmul(
                    bk[b], lhsT=ST[eb][:, i * P:(i + 1) * P], rhs=Yb[eb][:, nb * 512:(nb + 1) * 512],
                    start=True, stop=True), R=[r_ST[eb], r_Yb[eb]], W=[rb[b]])
                k.op("dve", lambda h, i=i, nb=nb, b=b: h.tensor_tensor(
                    out=xres[:, i, nb * 512:(nb + 1) * 512], in0=bk[b], in1=xres[:, i, nb * 512:(nb + 1) * 512],
                    op=ALU.add), R=[rb[b], r_x[i + 1]], W=[r_x[i + 1]])

        def prep(e):
            eb = e % 2
            Se = S[eb]

            def emit_S(h):
                ins = None
                for i in range(NT):
                    ins = h.tensor_scalar(out=Se[:, i, :], in0=iota_f[:, :], scalar1=rank[:, i, e:e + 1],
                                          scalar2=Afl[:, i, e:e + 1], op0=ALU.is_equal, op1=ALU.mult)
                return ins
            k.op("dve", emit_S, R=[r_rank, r_A, r_cf], W=[r_S[eb]])

            def emit_STt(h):
                ins = None
                for i in range(NT):
                    ins = h.transpose(bkb[6][:, i * P:(i + 1) * P], Se[:, i, :], ident_b[:, :])
                return ins
            k.op("pe", emit_STt, R=[r_S[eb], r_cb], W=[rb[6]])
            k.op("act", lambda h: h.copy(out=ST[eb][:, :], in_=bkb[6][:, :]), R=[rb[6]], W=[r_ST[eb]])

            def emit_ws(h):
                ins = None
                for i in range(NT):
                    ins = h.matmul(bk[7][:, 0:1], lhsT=Se[:, i, :], rhs=Wb[:, i, e:e + 1], start=(i == 0), stop=(i == NT - 1))
                return ins
            k.op("pe", emit_ws, R=[r_S[eb], r_W], W=[rb[7]])
            k.op("act", lambda h: h.copy(out=wslot[:, e:e + 1], in_=bk[7][:, 0:1]), R=[rb[7]], W=[r_ws])

        def gather(e, q):
            eb = e % 2
            Se, Xe = S[eb], XeT[eb]
            b = next_bank()

            def emit_g(h):
                ins = None
                for dk in range(4):
                    c = 4 * q + dk
                    for i in range(NT):
                        ins = h.matmul(bk[b][:, dk * P:(dk + 1) * P], lhsT=h2[:, i, c * P:(c + 1) * P], rhs=Se[:, i, :],
                                       start=(i == 0), stop=(i == NT - 1))
                return ins
            k.op("pe", emit_g, R=[r_S[eb]] + r_h2, W=[rb[b]])
            k.op("act", lambda h: h.copy(out=Xe[:, 4 * q:4 * q + 4, :], in_=bk[b].rearrange("p (k s) -> p k s", k=4)),
                 R=[rb[b]], W=[r_XeT[eb]])

        def gu_slab(e, kind, hh):
            Xe = XeT[e % 2]
            bb0 = 0 if kind == "g" else 2
            s = next_slab()
            wv = ring[s].rearrange("p (k n) -> p k n", k=8)

            def emit_gu(h):
                ins = None
                for k8 in range(8):
                    kk = hh * 8 + k8
                    for nh in range(2):
                        ins = h.matmul(bk[bb0 + nh], lhsT=Xe[:, kk, :], rhs=wv[:, k8, nh * 512:(nh + 1) * 512],
                                       start=(kk == 0), stop=(kk == KD - 1))
                return ins
            k.op("pe", emit_gu, R=[r_ring[s], r_XeT[e % 2]], W=[rb[bb0], rb[bb0 + 1]])
            issue_slab()

        def down_slab(e, dh):
            eb = e % 2
            s = next_slab()
            wv = ring[s].rearrange("p (c n) -> p c n", c=KH)
            for nn in range(2):
                nb = 2 * dh + nn
                b = next_bank()

                def emit_d(h, nn=nn, b=b):
                    ins = None
                    for c in range(KH):
                        ins = h.matmul(bk[b], lhsT=hidT[:, c, :], rhs=wv[:, c, nn * 512:(nn + 1) * 512],
                                       start=(c == 0), stop=(c == KH - 1))
                    return ins
                k.op("pe", emit_d, R=[r_ring[s], r_hidT], W=[rb[b]])
                k.op("dve", lambda h, nb=nb, b=b: h.tensor_scalar(
                    out=Yb[eb][:, nb * 512:(nb + 1) * 512], in0=bk[b], scalar1=wslot[:, e:e + 1], scalar2=None,
                    op0=ALU.mult), R=[rb[b], r_ws], W=[r_Yb[eb]])
            issue_slab()

        prep(0)
        for q in range(4):
            gather(0, q)
        for e in range(n_exp):
            has_prev, has_next = e > 0, e + 1 < n_exp
            gu_slab(e, "g", 0)
            if has_prev:
                combine_groups(e - 1, 0, 11)
            gu_slab(e, "g", 1)
            for nh in range(2):
                k.op("act", lambda h, nh=nh: h.activation(out=sg[nh][:, :], in_=bk[nh], func=AF.Silu),
                     R=[rb[nh]], W=[r_sg[nh]])
            if has_prev:
                combine_groups(e - 1, 11, 22)
            gu_slab(e, "u", 0)
            if has_prev:
                combine_groups(e - 1, 22, 32)
            if has_next:
                prep(e + 1)
            gu_slab(e, "u", 1)
            for nh in range(2):
                k.op("dve", lambda h, nh=nh: h.tensor_tensor(out=hid[:, nh * 512:(nh + 1) * 512], in0=bk[2 + nh],
                                                               in1=sg[nh][:, :], op=ALU.mult),
                     R=[rb[2 + nh], r_sg[nh]], W=[r_hid])

            def emit_hT(h):
                ins = None
                for c in range(KH):
                    ins = h.transpose(bkb[6][:, c * P:(c + 1) * P], hid[:, c * P:(c + 1) * P], ident_b[:, :])
                return ins
            k.op("pe", emit_hT, R=[r_hid, r_cb], W=[rb[6]])
            k.op("act", lambda h: h.copy(out=hidT, in_=bkb[6][:, :].rearrange("p (c s) -> p c s", c=KH)),
                 R=[rb[6]], W=[r_hidT])
            down_slab(e, 0)
            if has_next:
                gather(e + 1, 0)
                gather(e + 1, 1)
            down_slab(e, 1)
            if has_next:
                gather(e + 1, 2)
                gather(e + 1, 3)
        combine_groups(n_exp - 1, 0, 32)

        obuf = [arena_t[:, ring_off // 4 + j * D:ring_off // 4 + (j + 1) * D] for j in range(4)]
        NOB = len(obuf)
        gb3 = arena_t[:, ring_off // 4 + 4 * D:ring_off // 4 + 5 * D]
        junk2 = arena_t[:, ring_off // 4 + 5 * D:ring_off // 4 + 5 * D + D // 2].bitcast(BF16)
        ring_users = [t for r in r_ring[0:3] for t in (list(r.r) + [r.w])]
        r_gb3 = Res()
        k.dma("sp", gb3, g3.partition_broadcast(P), gb_ds, W=[r_gb3], extra=ring_users)
        yb0_users = ring_users
        r_ob = [Res() for _ in range(NOB)]
        for i in range(NT):
            k.op("act", lambda h, i=i: h.activation(out=junk2[:, :], in_=xres[:, i, :], func=AF.Square,
                                                    accum_out=ss[:, 20 + i:21 + i]), R=[r_x[i + 1]], W=[r_ss], extra=yb0_users)
        k.op("act", lambda h: h.activation(out=rs[:, 20:28], in_=ss[:, 20:28], func=AF.Sqrt, scale=1.0 / D, bias=EPS),
             R=[r_ss], W=[r_rs])
        k.op("dve", lambda h: h.reciprocal(out=rstd[:, 20:28], in_=rs[:, 20:28]), R=[r_rs], W=[r_rstd])
        dso = [k.dsem("o%d" % s) for s in range(NOB)]
        otoks = []
        for i in range(NT):
            s = i % NOB
            k.op("dve", lambda h, i=i, s=s: h.scalar_tensor_tensor(
                out=obuf[s][:, :], in0=xres[:, i, :], scalar=rstd[:, 20 + i:21 + i], in1=gb3,
                op0=ALU.mult, op1=ALU.mult), R=[r_x[i + 1], r_rstd, r_gb3], W=[r_ob[s]],
                extra=ring_users)
            otoks.append(k.dma("sp", out[i * P:(i + 1) * P, :], obuf[s][:, :], dso[s], R=[r_ob[s]]))
        k._wait(k.engs["sp"], k.all_tokens())
    return nc


_CACHE = {}


def _consts():
    ident = np.eye(P, dtype=np.float32)
    iota = np.broadcast_to(np.arange(P, dtype=np.float32)[None, :], (P, P)).copy()
    tri = (np.arange(P)[:, None] < np.arange(P)[None, :]).astype(np.float32)
    ones = np.ones((P, P), dtype=np.float32)
    return ident, iota, tri, ones


def make_in_maps(x, norm_mix_g, w_in, w_pool, pool_scale, conv_w, w_out, norm_ffn_g,
                 w_router_group, b_router_group, w_router_expert, b_router_expert,
                 w_gate, w_up, w_down, norm_final_g):
    f = lambda a: np.ascontiguousarray(np.asarray(a, dtype=np.float32))
    x = f(x)[0]
    ident, iota, tri, ones = _consts()
    xpad = np.concatenate([np.zeros((HALO, D), np.float32), x], axis=0)
    pscale = f(f(pool_scale)[0].reshape(8, P).T)
    convw = f(f(conv_w)[0].reshape(8, P, 3).transpose(1, 0, 2).reshape(P, 24))
    w_r = f(np.concatenate([f(w_router_group)[0], f(w_router_expert)[0]], axis=1))
    b_r = f(np.broadcast_to(np.concatenate([f(b_router_group)[0], f(b_router_expert)[0]])[None, :], (P, 36)))
    shared = {
        "g1": f(norm_mix_g)[0], "g2": f(norm_ffn_g)[0], "g3": f(norm_final_g),
        "w_in": f(w_in)[0], "w_pool": f(w_pool)[0], "w_out": f(w_out)[0],
        "pscale": pscale, "convw": convw, "w_r": w_r, "b_r": b_r,
        "w_gate": f(w_gate)[0], "w_up": f(w_up)[0], "w_down": f(w_down)[0],
        "ident": ident, "iota": iota, "tri": tri, "ones": ones,
    }
    in_maps = []
    for c in range(NCORES):
        t0 = c * TOK
        xh = np.ascontiguousarray(xpad[t0:t0 + TX])
        pos = t0 + np.arange(HALO) + 1
        inv = np.stack([1.0 / np.minimum(pos, w) for w in (2, 4, 8, 16)]).astype(np.float32)
        invcnt = f(np.broadcast_to(inv.reshape(1, 64), (P, 64)))
        m = dict(shared)
        m["xh"] = xh
        m["invcnt"] = invcnt
        in_maps.append(m)
    return in_maps


def kernel(**inputs):
    in_maps = make_in_maps(**inputs)
    if "nc" not in _CACHE:
        _CACHE["nc"] = build_nc()
    res = run_bass_kernel_spmd(_CACHE["nc"], in_maps, core_ids=list(range(NCORES)))
    outs = [np.asarray(r["out"], dtype=np.float32) for r in res.results]
    return np.concatenate(outs, axis=0).reshape(1, NCORES * TOK, D)
```

```python
import numpy as np
from contextlib import ExitStack
import concourse.bass as bass
import concourse.mybir as mybir
from concourse.bass_utils import run_bass_kernel_spmd

F32 = mybir.dt.float32
BF16 = mybir.dt.bfloat16
AF = mybir.ActivationFunctionType
ALU = mybir.AluOpType
AX = mybir.AxisListType

P = 128
D = 2048
KD = 16
NT = 8
TOK = 1024
HALO = 16
TX = TOK + HALO
NE = 32
DE = 1024
KH = 8
NCORES = 8
EPS = 1e-6
BIG = 1.0e30
NRING = 4
NWIN = 3
MAXFLY = 8


class Tok:
    __slots__ = ("sem", "val", "key")

    def __init__(self, sem, val, key):
        self.sem, self.val, self.key = sem, val, key


class Res:
    __slots__ = ("w", "r", "const")

    def __init__(self, const=False):
        self.w = None
        self.r = []
        self.const = const


class DSem:
    def __init__(self, sem, key):
        self.sem, self.key, self.count = sem, key, 0


class Eng:
    def __init__(self, name, h, sem):
        self.name, self.h, self.sem = name, h, sem
        self.count = 0
        self.waited = {}


class Tracker:
    def __init__(self, nc, es):
        self.nc, self.es = nc, es
        self.engs = {}
        for name, h in (("pe", nc.tensor), ("act", nc.scalar), ("dve", nc.vector),
                        ("pool", nc.gpsimd), ("sp", nc.sync)):
            self.engs[name] = Eng(name, h, es.enter_context(nc.semaphore("e_" + name)))
        self.dsems = []

    def dsem(self, name):
        d = DSem(self.es.enter_context(self.nc.semaphore("d_" + name)), "d_" + name)
        self.dsems.append(d)
        return d

    def _wait(self, E, deps):
        need = {}
        for d in deps:
            if d is None:
                continue
            cur = need.get(d.key)
            if cur is None or cur.val < d.val:
                need[d.key] = d
        for key, d in need.items():
            if E.waited.get(key, 0) < d.val:
                E.h.wait_ge(d.sem, d.val)
                E.waited[key] = d.val

    @staticmethod
    def _deps(R, W, extra):
        deps = list(extra)
        for r in R:
            deps.append(r.w)
        for w in W:
            deps.append(w.w)
            deps.extend(w.r)
        return deps

    @staticmethod
    def _commit(tok, R, W):
        for r in R:
            if not r.const:
                r.r.append(tok)
        for w in W:
            w.w = tok
            w.r = []

    def op(self, eng, emit, R=(), W=(), extra=()):
        E = self.engs[eng]
        self._wait(E, self._deps(R, W, extra))
        ins = emit(E.h)
        E.count += 1
        ins.then_inc(E.sem, 1)
        tok = Tok(E.sem, E.count, E.name)
        self._commit(tok, R, W)
        return tok

    def dma(self, queue, out, in_, ds, R=(), W=(), extra=(), nodep=False):
        E = self.engs[queue]
        if not nodep:
            self._wait(E, self._deps(R, W, extra))
        E.h.dma_start(out=out, in_=in_).then_inc(ds.sem, 16)
        ds.count += 16
        tok = Tok(ds.sem, ds.count, ds.key)
        self._commit(tok, R, W)
        return tok

    def all_tokens(self):
        toks = []
        for E in self.engs.values():
            if E.count > 0:
                toks.append(Tok(E.sem, E.count, E.name))
        for d in self.dsems:
            if d.count > 0:
                toks.append(Tok(d.sem, d.count, d.key))
        return toks

    def barrier(self):
        toks = self.all_tokens()
        for E in self.engs.values():
            self._wait(E, toks)


class Arena:
    def __init__(self, t, nbytes):
        self.t, self.nbytes, self.off = t, nbytes, 0

    def alloc(self, nelem, dtype):
        size = 4 if dtype == F32 else 2
        nb = (nelem * size + 63) // 64 * 64
        off = self.off
        self.off += nb
        assert self.off <= self.nbytes, ("arena overflow", self.off, self.nbytes)
        ap = self.t[:, off // 4:(off + nb) // 4]
        if dtype != F32:
            ap = ap.bitcast(dtype)
        return ap[:, 0:nelem]


ARENA_BYTES = 212480


def build_nc(dbg=False, stop_after=None):
    nc = bass.Bass("TRN2", target_bir_lowering=False)
    dt = nc.dram_tensor
    xh = dt("xh", [TX, D], F32, kind="ExternalInput").ap()
    g1 = dt("g1", [D], F32, kind="ExternalInput").ap()
    g2 = dt("g2", [D], F32, kind="ExternalInput").ap()
    g3 = dt("g3", [D], F32, kind="ExternalInput").ap()
    w_in = dt("w_in", [D, 4096], F32, kind="ExternalInput").ap()
    w_pool = dt("w_pool", [4, 256, 256], F32, kind="ExternalInput").ap()
    w_out = dt("w_out", [D, D], F32, kind="ExternalInput").ap()
    pscale_d = dt("pscale", [P, 8], F32, kind="ExternalInput").ap()
    convw_d = dt("convw", [P, 24], F32, kind="ExternalInput").ap()
    invcnt_d = dt("invcnt", [P, 64], F32, kind="ExternalInput").ap()
    w_r = dt("w_r", [D, 36], F32, kind="ExternalInput").ap()
    b_r = dt("b_r", [P, 36], F32, kind="ExternalInput").ap()
    w_gate = dt("w_gate", [NE, D, DE], F32, kind="ExternalInput").ap()
    w_up = dt("w_up", [NE, D, DE], F32, kind="ExternalInput").ap()
    w_down = dt("w_down", [NE, DE, D], F32, kind="ExternalInput").ap()
    ident_d = dt("ident", [P, P], F32, kind="ExternalInput").ap()
    iota_d = dt("iota", [P, P], F32, kind="ExternalInput").ap()
    tri_d = dt("tri", [P, P], F32, kind="ExternalInput").ap()
    ones_d = dt("ones", [P, P], F32, kind="ExternalInput").ap()
    out = dt("out", [TOK, D], F32, kind="ExternalOutput").ap()
    if dbg:
        dbg_x1 = dt("dbg_x1", [TOK, D], F32, kind="ExternalOutput").ap()
        dbg_rt = dt("dbg_rt", [P, 3 * NT * NE], F32, kind="ExternalOutput").ap()

    with ExitStack() as es:
        arena_t = es.enter_context(nc.sbuf_tensor("arena", [P, ARENA_BYTES // 4], F32))
        banks = [es.enter_context(nc.psum_tensor("pb%d" % i, [P, 512], F32)) for i in range(8)]
        bk = [b[:, :] for b in banks]
        bkb = [b[:, :].bitcast(BF16) for b in banks]
        rb = [Res() for _ in range(8)]
        k = Tracker(nc, es)
        A = Arena(arena_t, ARENA_BYTES)

        xres = A.alloc(NT * D, F32).rearrange("p (i d) -> p i d", i=NT)
        gb = A.alloc(D, F32)
        hT_flat = A.alloc(KD * TX, BF16)
        hT = hT_flat.rearrange("p (k t) -> p k t", k=KD)
        h2 = hT_flat[:, 0:NT * D].rearrange("p (i d) -> p i d", i=NT)
        ident_f = A.alloc(P, F32)
        iota_f = A.alloc(P, F32)
        ident_b = A.alloc(P, BF16)
        tri_b = A.alloc(P, BF16)
        ones_b = A.alloc(P, BF16)
        wr_sb = A.alloc(KD * 36, F32).rearrange("p (k n) -> p k n", k=KD)
        br = A.alloc(36, F32)
        pscale = A.alloc(8, F32)
        convw = A.alloc(24, F32).rearrange("p (j c) -> p j c", j=8)
        invcnt = A.alloc(64, F32).rearrange("p (g t) -> p g t", g=4)
        ss = A.alloc(32, F32)
        rs = A.alloc(32, F32)
        rstd = A.alloc(32, F32)
        Afl = A.alloc(NT * NE, F32).rearrange("p (i e) -> p i e", i=NT)
        Abf = A.alloc(NT * NE, BF16).rearrange("p (i e) -> p i e", i=NT)
        Wb = A.alloc(NT * NE, BF16).rearrange("p (i e) -> p i e", i=NT)
        Wfl = A.alloc(NT * NE, F32).rearrange("p (i e) -> p i e", i=NT)
        rank = A.alloc(NT * NE, F32).rearrange("p (i e) -> p i e", i=NT)
        wslot = A.alloc(NE, F32)
        phase_mark = A.off

        r_cf = Res(const=True)
        r_cb = Res(const=True)
        r_gb = Res()
        r_x = [Res() for _ in range(NT + 1)]
        r_hT = Res()
        r_ss, r_rs, r_rstd = Res(), Res(), Res()

        cd_sp = k.dsem("c_sp")
        cd_pl = k.dsem("c_pl")
        k.dma("sp", gb, g1.partition_broadcast(P), k.dsem("gb"), W=[r_gb], nodep=True)
        gb_ds = k.dsems[-1]

        def load_consts_sp():
            for dst, src in ((ident_f, ident_d[:, :]), (iota_f, iota_d[:, :]), (br, b_r[:, :]),
                             (pscale, pscale_d[:, :]), (convw, convw_d.rearrange("p (j c) -> p j c", j=8)),
                             (invcnt, invcnt_d.rearrange("p (g t) -> p g t", g=4)),
                             (wr_sb, w_r.rearrange("(k p) n -> p k n", p=P))):
                t = k.dma("sp", dst, src, cd_sp, nodep=True)
            r_cf.w = t

        mixT = A.alloc(KD * TOK, BF16).rearrange("p (c t) -> p c t", c=KD)
        win = [A.alloc(KD * P, BF16).rearrange("p (k c) -> p k c", k=KD) for _ in range(NWIN)]
        fb = [A.alloc(TX, F32) for _ in range(4)]
        tA = A.alloc(TX, F32)
        tB = A.alloc(TX, F32)
        hn_off = A.off
        hn = [A.alloc(D, BF16) for _ in range(2)]
        pooledT = [A.alloc(2 * TOK, BF16).rearrange("p (k t) -> p k t", k=2) for _ in range(2)]
        wpool_b = A.alloc(4 * 2 * 256, BF16).rearrange("p (g k d) -> p g k d", g=4, k=2)
        c16 = A.alloc(16, F32)
        xhalo_flat = arena_t[:, (phase_mark + KD * TOK * 2 + NWIN * KD * P * 2) // 4:
                             (phase_mark + KD * TOK * 2 + NWIN * KD * P * 2) // 4 + D]
        xhalo = xhalo_flat

        for dst, src in ((ident_b, ident_d[:, :]), (tri_b, tri_d[:, :]), (ones_b, ones_d[:, :]),
                         (wpool_b, w_pool.rearrange("g (k p) d -> p g k d", p=P))):
            t = k.dma("pool", dst, src, cd_pl, nodep=True)
        r_cb.w = t

        dsx = [k.dsem("x%d" % i) for i in range(NT + 1)]
        xsrc = [None] * (NT + 1)
        npt = [HALO] + [P] * NT
        xtoks = []
        for ti in list(range(1, NT + 1)) + [0]:
            if len(xtoks) >= 2:
                k._wait(k.engs["sp"], [xtoks[-2]])
            if ti == 0:
                xsrc[ti] = xhalo[0:HALO, :]
                xtoks.append(k.dma("sp", xsrc[ti], xh[0:HALO, :], dsx[ti], W=[r_x[ti]], nodep=True))
            else:
                xsrc[ti] = xres[:, ti - 1, :]
                xtoks.append(k.dma("sp", xsrc[ti], xh[HALO + (ti - 1) * P:HALO + ti * P, :], dsx[ti], W=[r_x[ti]],
                                   nodep=True))
        load_consts_sp()

        k.op("dve", lambda h: h.memset(ss[:, :], 1.0), W=[r_ss])
        r_hn = [Res(), Res()]
        r_tA = Res()
        sqjunk = tA[:, 0:1024].bitcast(BF16)

        def norm_stats(ti):
            n = npt[ti]
            k.op("act", lambda h: h.activation(out=sqjunk[0:n, :], in_=xsrc[ti], func=AF.Square,
                                               accum_out=ss[0:n, ti:ti + 1]),
                 R=[r_x[ti]], W=[r_ss, r_tA])
            k.op("act", lambda h: h.activation(out=rs[:, ti:ti + 1], in_=ss[:, ti:ti + 1], func=AF.Sqrt,
                                               scale=1.0 / D, bias=EPS), R=[r_ss], W=[r_rs])
            k.op("dve", lambda h: h.reciprocal(out=rstd[:, ti:ti + 1], in_=rs[:, ti:ti + 1]), R=[r_rs], W=[r_rstd])

        seq = list(range(1, NT + 1)) + [0]
        pos_of = {ti: pos for pos, ti in enumerate(seq)}

        def tp_bank(ti, half):
            return (6 if pos_of[ti] % 2 == 0 else 4) + half

        def norm_apply(ti):
            n = npt[ti]
            par = pos_of[ti] % 2
            hb = hn[par]
            k.op("dve", lambda h: h.scalar_tensor_tensor(
                out=hb[0:n, :], in0=xsrc[ti], scalar=rstd[0:n, ti:ti + 1], in1=gb[0:n, :],
                op0=ALU.mult, op1=ALU.mult), R=[r_x[ti], r_rstd, r_gb], W=[r_hn[par]])
            for half in range(2):
                b = tp_bank(ti, half)

                def emit_tp(h, half=half, b=b):
                    ins = None
                    for j in range(8):
                        c = half * 8 + j
                        ins = h.transpose(bkb[b][:, j * n:(j + 1) * n], hb[0:n, c * P:(c + 1) * P], ident_b[0:n, 0:n])
                    return ins
                k.op("pe", emit_tp, R=[r_hn[par], r_cb], W=[rb[b]])

        def norm_evac(ti):
            n = npt[ti]
            toff = 0 if ti == 0 else HALO + (ti - 1) * P
            for half in range(2):
                b = tp_bank(ti, half)
                dst = hT[:, half * 8:half * 8 + 8, toff:toff + n]
                srcp = bkb[b][:, 0:8 * n].rearrange("p (k n) -> p k n", k=8)
                if half == 0:
                    k.op("act", lambda h, dst=dst, srcp=srcp: h.copy(out=dst, in_=srcp), R=[rb[b]], W=[r_hT])
                else:
                    k.op("dve", lambda h, dst=dst, srcp=srcp: h.tensor_copy(out=dst, in_=srcp), R=[rb[b]], W=[r_hT])

        for ti in seq[0:3]:
            norm_stats(ti)
        for pos, ti in enumerate(seq):
            norm_apply(ti)
            if pos >= 1:
                norm_evac(seq[pos - 1])
            if pos + 3 < len(seq):
                norm_stats(seq[pos + 3])
        norm_evac(seq[-1])
        k.dma("sp", gb, g2.partition_broadcast(P), gb_ds, W=[r_gb])

        order = list(range(8)) + [c for j in range(8) for c in (8 + j, 16 + j, 24 + j)]
        dswin = [k.dsem("win%d" % s) for s in range(NWIN)]
        r_win = [Res() for _ in range(NWIN)]
        r_fb = [Res() for _ in range(4)]
        r_tB, r_c16 = Res(), Res()
        r_pooled = [Res(), Res()]
        r_mix = [Res() for _ in range(KD)]
        pjsets = [(0, 1, 4), (2, 3, 5)]
        pjctr = [0]
        fbctr = [0]

        def win_dma(idx):
            cc = order[idx]
            s = idx % NWIN
            k.dma("pool", win[s], w_in[:, cc * P:(cc + 1) * P].rearrange("(k p) c -> p k c", p=P),
                  dswin[s], W=[r_win[s]])

        for idx in range(NWIN):
            win_dma(idx)

        dswo = [k.dsem("wo%d" % s) for s in range(2)]
        r_wo = [Res(), Res()]
        wout_sl = [arena_t[:, hn_off // 4:hn_off // 4 + 4096].bitcast(BF16).rearrange("p (k c) -> p k c", k=KD),
                   hT_flat[:, 0:8192].rearrange("p (k c) -> p k c", k=KD)]
        wout_extra = [[], []]

        def wout_dma(n):
            s = n % 2
            k.dma("pool", wout_sl[s], w_out[:, n * 512:(n + 1) * 512].rearrange("(k p) c -> p k c", p=P),
                  dswo[s], W=[r_wo[s]], extra=wout_extra[s])

        pending_pool = []

        def flush_pool():
            while pending_pool:
                gi, par = pending_pool.pop(0)
                for m in range(2):
                    pb0, pb1, _ = pjsets[pjctr[0] % 2]
                    pjctr[0] += 1

                    def emit_pm(h, gi=gi, m=m, par=par, pb0=pb0, pb1=pb1):
                        ins = None
                        for tb, pbb in ((0, pb0), (1, pb1)):
                            for kc2 in range(2):
                                ins = h.matmul(bk[pbb], lhsT=wpool_b[:, gi, kc2, m * P:(m + 1) * P],
                                               rhs=pooledT[par][:, kc2, tb * 512:(tb + 1) * 512],
                                               start=(kc2 == 0), stop=(kc2 == 1))
                        return ins
                    tpe = k.op("pe", emit_pm, R=[r_pooled[par], r_cb], W=[rb[pb0], rb[pb1]])
                    mc = 2 * gi + m

                    def emit_pe(h, mc=mc, pb0=pb0, pb1=pb1):
                        h.tensor_scalar(out=mixT[:, mc, 0:512], in0=bk[pb0], scalar1=pscale[:, mc:mc + 1],
                                        scalar2=None, op0=ALU.mult)
                        return h.tensor_scalar(out=mixT[:, mc, 512:1024], in0=bk[pb1], scalar1=pscale[:, mc:mc + 1],
                                               scalar2=None, op0=ALU.mult)
                    tdv = k.op("dve", emit_pe, R=[rb[pb0], rb[pb1], r_cf], W=[r_mix[mc]])
                if gi == 3:
                    wout_extra[0] = [tpe, tdv, r_cb.w]
                    wout_dma(0)

        conv_fb = {}
        for idx, cc in enumerate(order):
            s = idx % NWIN
            b0, b1, bh = pjsets[pjctr[0] % 2]
            pjctr[0] += 1

            def emit_proj(h, s=s, b0=b0, b1=b1, bh=bh):
                ins = None
                for kk in range(KD):
                    st, sp_ = (kk == 0), (kk == KD - 1)
                    h.matmul(bk[b0], lhsT=win[s][:, kk, :], rhs=hT[:, kk, HALO:HALO + 512], start=st, stop=sp_)
                    h.matmul(bk[b1], lhsT=win[s][:, kk, :], rhs=hT[:, kk, HALO + 512:TX], start=st, stop=sp_)
                    ins = h.matmul(bk[bh][:, 0:HALO], lhsT=win[s][:, kk, :], rhs=hT[:, kk, 0:HALO], start=st, stop=sp_)
                return ins
            k.op("pe", emit_proj, R=[r_win[s], r_hT], W=[rb[b0], rb[b1], rb[bh]])
            if idx + NWIN < len(order):
                win_dma(idx + NWIN)
            f = fbctr[0] % 4
            fbctr[0] += 1

            def emit_ev(h, f=f, b0=b0, b1=b1, bh=bh):
                h.copy(out=fb[f][:, HALO:HALO + 512], in_=bk[b0])
                h.copy(out=fb[f][:, HALO + 512:TX], in_=bk[b1])
                return h.copy(out=fb[f][:, 0:HALO], in_=bk[bh][:, 0:HALO])
            k.op("act", emit_ev, R=[rb[b0], rb[b1], rb[bh]], W=[r_fb[f]])
            flush_pool()

            if cc < 8:
                gi, kc = cc // 2, cc % 2
                w = 2 << gi
                par = gi % 2
                u = fb[f]
                cur, rcur, lo, shift = u, r_fb[f], 0, 1
                for l in range(gi + 1):
                    dst, rdst = (tA, r_tA) if l % 2 == 0 else (tB, r_tB)
                    lo2 = lo + shift
                    k.op("dve", lambda h, dst=dst, cur=cur, lo2=lo2, shift=shift: h.tensor_tensor(
                        out=dst[:, lo2:TX], in0=cur[:, lo2:TX], in1=cur[:, lo2 - shift:TX - shift], op=ALU.add),
                        R=[rcur], W=[rdst])
                    cur, rcur, lo, shift = dst, rdst, lo2, shift * 2
                k.op("dve", lambda h, cur=cur, u=u, par=par, kc=kc, w=w: h.scalar_tensor_tensor(
                    out=pooledT[par][:, kc, :], in0=cur[:, HALO:TX], scalar=1.0 / w, in1=u[:, HALO:TX],
                    op0=ALU.mult, op1=ALU.subtract), R=[rcur, r_fb[f]], W=[r_pooled[par]])
                k.op("dve", lambda h, cur=cur, gi=gi: h.tensor_tensor(
                    out=c16[:, :], in0=cur[:, HALO:2 * HALO], in1=invcnt[:, gi, :], op=ALU.mult),
                    R=[rcur, r_cf], W=[r_c16])
                k.op("dve", lambda h, u=u, par=par, kc=kc: h.tensor_tensor(
                    out=pooledT[par][:, kc, 0:HALO], in0=c16[:, :], in1=u[:, HALO:2 * HALO], op=ALU.subtract),
                    R=[r_c16, r_fb[f]], W=[r_pooled[par]])
                if kc == 1:
                    pending_pool.append((gi, par))
            else:
                j = (cc - 8) % 8
                conv_fb.setdefault(j, {})[(cc - 8) // 8] = f
                if cc >= 24:
                    fbb, fc, fv = conv_fb[j][0], conv_fb[j][1], conv_fb[j][2]
                    k.op("dve", lambda h, fc=fc, fv=fv: h.tensor_tensor(
                        out=tA[:, 14:TX], in0=fb[fc][:, 14:TX], in1=fb[fv][:, 14:TX], op=ALU.mult),
                        R=[r_fb[fc], r_fb[fv]], W=[r_tA])
                    k.op("dve", lambda h, j=j: h.tensor_scalar(
                        out=tB[:, HALO:TX], in0=tA[:, 14:TX - 2], scalar1=convw[:, j, 0:1], scalar2=None, op0=ALU.mult),
                        R=[r_tA, r_cf], W=[r_tB])
                    k.op("dve", lambda h, j=j: h.scalar_tensor_tensor(
                        out=tB[:, HALO:TX], in0=tA[:, 15:TX - 1], scalar=convw[:, j, 1:2], in1=tB[:, HALO:TX],
                        op0=ALU.mult, op1=ALU.add), R=[r_tA, r_tB, r_cf], W=[r_tB])
                    k.op("dve", lambda h, j=j: h.scalar_tensor_tensor(
                        out=tB[:, HALO:TX], in0=tA[:, HALO:TX], scalar=convw[:, j, 2:3], in1=tB[:, HALO:TX],
                        op0=ALU.mult, op1=ALU.add), R=[r_tA, r_tB, r_cf], W=[r_tB])
                    k.op("dve", lambda h, j=j, fbb=fbb: h.tensor_tensor(
                        out=mixT[:, 8 + j, :], in0=fb[fbb][:, HALO:TX], in1=tB[:, HALO:TX], op=ALU.mult),
                        R=[r_fb[fbb], r_tB], W=[r_mix[8 + j]])

        hT_users = list(r_hT.r) + [r_hT.w]
        wout_extra[1] = hT_users
        wout_dma(1)
        bctr = 0
        junk1c = fb[0][:, 0:1024].bitcast(BF16)
        for n in range(4):
            s = n % 2
            for i in range(NT):
                b = bctr % 4
                bctr += 1

                def emit_op(h, s=s, i=i, b=b):
                    ins = None
                    for kk in range(KD):
                        ins = h.matmul(bk[b], lhsT=mixT[:, kk, i * P:(i + 1) * P], rhs=wout_sl[s][:, kk, :],
                                       start=(kk == 0), stop=(kk == KD - 1))
                    return ins
                k.op("pe", emit_op, R=[r_wo[s]] + r_mix, W=[rb[b]])
                k.op("dve", lambda h, i=i, n=n, b=b: h.tensor_tensor(
                    out=xres[:, i, n * 512:(n + 1) * 512], in0=bk[b], in1=xres[:, i, n * 512:(n + 1) * 512], op=ALU.add),
                    R=[rb[b], r_x[i + 1]], W=[r_x[i + 1]])
                if n == 3:
                    k.op("act", lambda h, i=i: h.activation(out=junk1c, in_=xres[:, i, :], func=AF.Square,
                                                            accum_out=ss[:, 10 + i:11 + i]),
                         R=[r_x[i + 1]], W=[r_ss, r_fb[0]])
            if n + 2 < 4:
                wout_dma(n + 2)

        if dbg:
            dd = k.dsem("dbg")
            for i in range(NT):
                k.dma("sp", dbg_x1[i * P:(i + 1) * P, :], xres[:, i, :], dd, R=[r_x[i + 1]])

        k.barrier()
        if stop_after == "1C":
            return nc

        ring_off = ARENA_BYTES - NRING * 16384
        ring = [arena_t[:, (ring_off + s * 16384) // 4:(ring_off + (s + 1) * 16384) // 4].bitcast(BF16)
                for s in range(NRING)]
        dsr = [k.dsem("ring%d" % s) for s in range(NRING)]
        r_ring = [Res() for _ in range(NRING)]
        n_exp = NE if stop_after != "E1" else 4
        slabs = []
        for e in range(n_exp):
            for kind in ("g", "u"):
                slabs.append((kind, e, 0))
                slabs.append((kind, e, 1))
            slabs.append(("d", e, 0))
            slabs.append(("d", e, 1))
        slab_ptr = [0]
        slab_toks = []

        def issue_slab():
            if slab_ptr[0] >= len(slabs):
                return
            kind, e, hh = slabs[slab_ptr[0]]
            s = (slab_ptr[0] + 1) % NRING
            slab_ptr[0] += 1
            if kind == "d":
                dst = ring[s].rearrange("p (c n) -> p c n", c=KH)
                src = w_down[e, :, hh * 1024:(hh + 1) * 1024].rearrange("(c p) n -> p c n", p=P)
            else:
                wsrc = w_gate if kind == "g" else w_up
                dst = ring[s].rearrange("p (k n) -> p k n", k=8)
                src = wsrc[e].rearrange("(p k) n -> p k n", p=P)[:, hh * 8:(hh + 1) * 8, :]
            gate = [slab_toks[-MAXFLY]] if len(slab_toks) >= MAXFLY else []
            slab_toks.append(k.dma("pool", dst, src, dsr[s], W=[r_ring[s]], extra=gate))

        for _ in range(NRING - 1):
            issue_slab()

        A.off = phase_mark
        h2f_l = [A.alloc(D, F32) for _ in range(2)]
        h2fT_l = [A.alloc(KD * P, F32).rearrange("p (k t) -> p k t", k=KD) for _ in range(2)]
        junk = A.alloc(D, BF16)
        lg_all = A.alloc(NT * 36, F32).rearrange("p (i n) -> p i n", i=NT)
        r_h2f_l, r_h2fT_l = [Res(), Res()], [Res(), Res()]
        r_h2 = [Res() for _ in range(NT)]
        r_A, r_W, r_rank = Res(), Res(), Res()
        r_lg = Res()

        k.op("act", lambda h: h.activation(out=rs[:, 10:18], in_=ss[:, 10:18], func=AF.Sqrt, scale=1.0 / D, bias=EPS),
             R=[r_ss], W=[r_rs])
        k.op("dve", lambda h: h.reciprocal(out=rstd[:, 10:18], in_=rs[:, 10:18]), R=[r_rs], W=[r_rstd])

        def stage_a1(i):
            h2f, h2fT = h2f_l[i % 2], h2fT_l[i % 2]
            r_h2f, r_h2fT = r_h2f_l[i % 2], r_h2fT_l[i % 2]
            k.op("dve", lambda h: h.scalar_tensor_tensor(
                out=h2f[:, :], in0=xres[:, i, :], scalar=rstd[:, 10 + i:11 + i], in1=gb[:, :],
                op0=ALU.mult, op1=ALU.mult), R=[r_x[i + 1], r_rstd, r_gb], W=[r_h2f])
            k.op("pool", lambda h: h.tensor_copy(out=h2[:, i, :].rearrange("t (k p) -> t k p", p=P),
                                                 in_=h2f[:, :].rearrange("t (p k) -> t k p", k=KD)),
                 R=[r_h2f], W=[r_h2[i]])
            for q in range(4):
                def emit_tf(h, q=q):
                    ins = None
                    for j in range(4):
                        c = 4 * q + j
                        ins = h.transpose(bk[q][:, j * P:(j + 1) * P], h2f[:, c * P:(c + 1) * P], ident_f[:, :])
                    return ins
                k.op("pe", emit_tf, R=[r_h2f, r_cf], W=[rb[q]])
                ev_eng = "act" if q % 2 == 0 else "dve"
                if ev_eng == "act":
                    k.op("act", lambda h, q=q: h.copy(out=h2fT[:, 4 * q:4 * q + 4, :],
                                                      in_=bk[q].rearrange("p (k n) -> p k n", k=4)),
                         R=[rb[q]], W=[r_h2fT])
                else:
                    k.op("dve", lambda h, q=q: h.tensor_copy(out=h2fT[:, 4 * q:4 * q + 4, :],
                                                             in_=bk[q].rearrange("p (k n) -> p k n", k=4)),
                         R=[rb[q]], W=[r_h2fT])

        def stage_a2(i):
            h2fT = h2fT_l[i % 2]
            r_h2fT = r_h2fT_l[i % 2]
            lgb = 4 + 2 * (i % 2)

            def emit_lg(h):
                ins = None
                for kk in range(KD):
                    ins = h.matmul(bk[lgb][:, 0:36], lhsT=h2fT[:, kk, :], rhs=wr_sb[:, kk, :],
                                   start=(kk == 0), stop=(kk == KD - 1))
                return ins
            k.op("pe", emit_lg, R=[r_h2fT, r_cf], W=[rb[lgb]])
            k.op("dve", lambda h: h.tensor_tensor(out=lg_all[:, i, :], in0=bk[lgb][:, 0:36], in1=br[:, :], op=ALU.add),
                 R=[rb[lgb], r_cf], W=[r_lg])

        stage_a1(0)
        for i in range(NT):
            if i + 1 < NT:
                stage_a1(i + 1)
            stage_a2(i)

        def al(n):
            return A.alloc(n, F32)
        gmax, gsum, gw = al(NT), al(NT), al(NT)
        gsh, gexp, gmask, pen = al(NT * 4), al(NT * 4), al(NT * 4), al(NT * 4)
        em, mask1, em2, mask2, w1m, w2m = (al(NT * NE) for _ in range(6))
        top1, top2, dlt, ed, den, p1, wt1, wt2 = (al(NT) for _ in range(8))
        v3 = lambda ap_, n: ap_.rearrange("p (i n) -> p i n", i=NT)
        bc = lambda ap_, n: ap_.unsqueeze(2).to_broadcast([P, NT, n])
        gl = lg_all[:, :, 0:4]
        el = lg_all[:, :, 4:36]
        rr = {n_: Res() for n_ in ("gmax", "gsh", "gexp", "gsum", "gw", "gmask", "pen", "em", "mask1", "em2", "mask2",
                                   "top1", "top2", "dlt", "ed", "den", "p1", "wt1", "wt2", "w1m", "w2m")}
        dv = lambda emit, R, W: k.op("dve", emit, R=R, W=W)
        dv(lambda h: h.reduce_max(out=gmax, in_=gl, axis=AX.X), [r_lg], [rr["gmax"]])
        dv(lambda h: h.tensor_tensor(out=v3(gsh, 4), in0=gl, in1=bc(gmax, 4), op=ALU.subtract), [r_lg, rr["gmax"]], [rr["gsh"]])
        k.op("act", lambda h: h.activation(out=gexp, in_=gsh, func=AF.Exp), R=[rr["gsh"]], W=[rr["gexp"]])
        dv(lambda h: h.reduce_sum(out=gsum, in_=v3(gexp, 4), axis=AX.X), [rr["gexp"]], [rr["gsum"]])
        dv(lambda h: h.reciprocal(out=gw, in_=gsum), [rr["gsum"]], [rr["gw"]])
        dv(lambda h: h.tensor_tensor(out=v3(gmask, 4), in0=gl, in1=bc(gmax, 4), op=ALU.is_equal), [r_lg, rr["gmax"]], [rr["gmask"]])
        dv(lambda h: h.tensor_scalar(out=pen, in0=gmask, scalar1=-1.0, scalar2=BIG, op0=ALU.add, op1=ALU.mult),
           [rr["gmask"]], [rr["pen"]])
        dv(lambda h: h.tensor_tensor(out=em.rearrange("p (i g j) -> p i g j", i=NT, g=4),
                                     in0=el.rearrange("p i (g j) -> p i g j", g=4),
                                     in1=v3(pen, 4).unsqueeze(3).to_broadcast([P, NT, 4, 8]), op=ALU.add),
           [r_lg, rr["pen"]], [rr["em"]])
        dv(lambda h: h.reduce_max(out=top1, in_=v3(em, NE), axis=AX.X), [rr["em"]], [rr["top1"]])
        def per_tile(out_, in_, sc, op):
            def emit(h):
                ins = None
                for i in range(NT):
                    ins = h.tensor_scalar(out=out_[:, i * NE:(i + 1) * NE], in0=in_[:, i * NE:(i + 1) * NE],
                                          scalar1=sc[:, i:i + 1], scalar2=None, op0=op)
                return ins
            return emit
        dv(per_tile(mask1, em, top1, ALU.is_equal), [rr["em"], rr["top1"]], [rr["mask1"]])
        dv(lambda h: h.scalar_tensor_tensor(out=em2, in0=mask1, scalar=-BIG, in1=em, op0=ALU.mult, op1=ALU.add),
           [rr["em"], rr["mask1"]], [rr["em2"]])
        dv(lambda h: h.reduce_max(out=top2, in_=v3(em2, NE), axis=AX.X), [rr["em2"]], [rr["top2"]])
        dv(per_tile(mask2, em2, top2, ALU.is_equal), [rr["em2"], rr["top2"]], [rr["mask2"]])
        dv(lambda h: h.tensor_tensor(out=dlt, in0=top2, in1=top1, op=ALU.subtract), [rr["top1"], rr["top2"]], [rr["dlt"]])
        k.op("act", lambda h: h.activation(out=ed, in_=dlt, func=AF.Exp), R=[rr["dlt"]], W=[rr["ed"]])
        dv(lambda h: h.tensor_scalar(out=den, in0=ed, scalar1=1.0, scalar2=None, op0=ALU.add), [rr["ed"]], [rr["den"]])
        dv(lambda h: h.reciprocal(out=p1, in_=den), [rr["den"]], [rr["p1"]])
        dv(lambda h: h.tensor_tensor(out=wt1, in0=p1, in1=gw, op=ALU.mult), [rr["p1"], rr["gw"]], [rr["wt1"]])
        dv(lambda h: h.tensor_tensor(out=wt2, in0=wt1, in1=ed, op=ALU.mult), [rr["wt1"], rr["ed"]], [rr["wt2"]])
        Afl2 = Afl.rearrange("p i e -> p (i e)")
        Wfl2 = Wfl.rearrange("p i e -> p (i e)")
        dv(lambda h: h.tensor_tensor(out=Afl2, in0=mask1, in1=mask2, op=ALU.add), [rr["mask1"], rr["mask2"]], [r_A])
        dv(lambda h: h.tensor_copy(out=Abf.rearrange("p i e -> p (i e)"), in_=Afl2), [r_A], [r_A])
        dv(per_tile(w1m, mask1, wt1, ALU.mult), [rr["mask1"], rr["wt1"]], [rr["w1m"]])
        dv(per_tile(w2m, mask2, wt2, ALU.mult), [rr["mask2"], rr["wt2"]], [rr["w2m"]])
        dv(lambda h: h.tensor_tensor(out=Wfl2, in0=w1m, in1=w2m, op=ALU.add), [rr["w1m"], rr["w2m"]], [r_W])
        dv(lambda h: h.tensor_copy(out=Wb.rearrange("p i e -> p (i e)"), in_=Wfl2), [r_W], [r_W])

        for i in range(NT):
            def emit_rk(h, i=i):
                ins = None
                for i2 in range(i):
                    h.matmul(bk[5][:, 0:NE], lhsT=ones_b[:, :], rhs=Abf[:, i2, :], start=(i2 == 0), stop=False)
                ins = h.matmul(bk[5][:, 0:NE], lhsT=tri_b[:, :], rhs=Abf[:, i, :], start=(i == 0), stop=True)
                return ins
            k.op("pe", emit_rk, R=[r_A, r_cb], W=[rb[5]])
            k.op("dve", lambda h, i=i: h.tensor_copy(out=rank[:, i, :], in_=bk[5][:, 0:NE]), R=[rb[5]], W=[r_rank])


        if dbg:
            dd2 = k.dsem("dbg2")
            k.dma("sp", dbg_rt[:, 0:NT * NE], Afl.rearrange("p i e -> p (i e)"), dd2, R=[r_A])
            k.dma("sp", dbg_rt[:, NT * NE:2 * NT * NE], Wfl.rearrange("p i e -> p (i e)"), dd2, R=[r_W])
            k.dma("sp", dbg_rt[:, 2 * NT * NE:3 * NT * NE], rank.rearrange("p i e -> p (i e)"), dd2, R=[r_rank])

        assert A.off <= ring_off + 16384, ("router-phase scratch runs into prefetched ring slots", A.off, ring_off)
        k.barrier()

        A.off = phase_mark
        gb_bf = gb.bitcast(BF16)
        Yb0 = gb_bf[:, 0:D]
        ST = [gb_bf[:, D + j * TOK:D + (j + 1) * TOK] for j in range(2)]
        ob_off = A.off
        XeT1 = A.alloc(KD * P, BF16).rearrange("p (k s) -> p k s", k=KD)
        XeT = [XeT1, XeT1]
        S0 = A.alloc(NT * P, BF16).rearrange("p (i s) -> p i s", i=NT)
        S = [S0, S0]
        hid = A.alloc(DE, BF16)
        hidT = A.alloc(KH * P, BF16).rearrange("p (c s) -> p c s", c=KH)
        sg = [wr_sb.rearrange("p k n -> p (k n)")[:, 0:512], A.alloc(512, F32)]
        Yb = [Yb0, A.alloc(D, BF16)]
        ST.append(A.alloc(TOK, BF16))
        ob_end = A.off
        assert A.off <= ring_off, ("expert-phase buffers run into the weight ring", A.off, ring_off)
        r_Yb = [Res(), Res()]
        r_ST = [Res(), Res(), Res()]
        r_XeT1 = Res()
        r_XeT = [r_XeT1, r_XeT1]
        r_S0 = Res()
        r_S = [r_S0, r_S0]
        r_hid, r_hidT = Res(), Res()
        r_sg = [Res(), Res()]
        r_ws = Res()

        issue_slab()
        use_ptr = [1]

        def next_slab():
            s = use_ptr[0] % NRING
            use_ptr[0] += 1
            return s

        rot = [0]

        def next_bank():
            b = 4 + rot[0] % 2
            rot[0] += 1
            return b

        def combine_groups(e0, lo, hi):
            for gidx in range(lo, hi):
                i, nb = gidx // 4, gidx % 4
                b = next_bank()

                def emit_c(h, i=i, nb=nb, b=b):
                    h.matmul(bk[b], lhsT=ST[e0 % 3][:, i * P:(i + 1) * P], rhs=Yb[e0 % 2][:, nb * 512:(nb + 1) * 512],
                             start=True, stop=False)
                    return h.matmul(bk[b], lhsT=ST[(e0 + 1) % 3][:, i * P:(i + 1) * P],
                                    rhs=Yb[(e0 + 1) % 2][:, nb * 512:(nb + 1) * 512], start=False, stop=True)
                k.op("pe", emit_c, R=[r_ST[e0 % 3], r_ST[(e0 + 1) % 3], r_Yb[0], r_Yb[1]], W=[rb[b]])
                k.op("dve", lambda h, i=i, nb=nb, b=b: h.tensor_tensor(
                    out=xres[:, i, nb * 512:(nb + 1) * 512], in0=bk[b], in1=xres[:, i, nb * 512:(nb + 1) * 512],
                    op=ALU.add), R=[rb[b], r_x[i + 1]], W=[r_x[i + 1]])

        def prep(e):
            eb = e % 2
            Se = S[eb]

            def emit_S(h):
                ins = None
                for i in range(NT):
                    ins = h.tensor_scalar(out=Se[:, i, :], in0=iota_f[:, :], scalar1=rank[:, i, e:e + 1],
                                          scalar2=Afl[:, i, e:e + 1], op0=ALU.is_equal, op1=ALU.mult)
                return ins
            k.op("dve", emit_S, R=[r_rank, r_A, r_cf], W=[r_S[eb]])

            def emit_STt(h):
                ins = None
                for i in range(NT):
                    ins = h.transpose(bkb[6][:, i * P:(i + 1) * P], Se[:, i, :], ident_b[:, :])
                return ins
            k.op("pe", emit_STt, R=[r_S[eb], r_cb], W=[rb[6]])
            k.op("act", lambda h: h.copy(out=ST[e % 3][:, :], in_=bkb[6][:, :]), R=[rb[6]], W=[r_ST[e % 3]])

            def emit_ws(h):
                ins = None
                for i in range(NT):
                    ins = h.matmul(bk[7][:, 0:1], lhsT=Se[:, i, :], rhs=Wb[:, i, e:e + 1], start=(i == 0), stop=(i == NT - 1))
                return ins
            k.op("pe", emit_ws, R=[r_S[eb], r_W], W=[rb[7]])
            k.op("act", lambda h: h.copy(out=wslot[:, e:e + 1], in_=bk[7][:, 0:1]), R=[rb[7]], W=[r_ws])

        def gather(e, q):
            eb = e % 2
            Se, Xe = S[eb], XeT[eb]
            b = next_bank()

            def emit_g(h):
                ins = None
                for dk in range(4):
                    c = 4 * q + dk
                    for i in range(NT):
                        ins = h.matmul(bk[b][:, dk * P:(dk + 1) * P], lhsT=h2[:, i, c * P:(c + 1) * P], rhs=Se[:, i, :],
                                       start=(i == 0), stop=(i == NT - 1))
                return ins
            k.op("pe", emit_g, R=[r_S[eb]] + r_h2, W=[rb[b]])
            k.op("act", lambda h: h.copy(out=Xe[:, 4 * q:4 * q + 4, :], in_=bk[b].rearrange("p (k s) -> p k s", k=4)),
                 R=[rb[b]], W=[r_XeT[eb]])

        def gu_slab(e, kind, hh):
            Xe = XeT[e % 2]
            bb0 = 0 if kind == "g" else 2
            s = next_slab()
            wv = ring[s].rearrange("p (k n) -> p k n", k=8)

            def emit_gu(h):
                ins = None
                for k8 in range(8):
                    kk = hh * 8 + k8
                    for nh in range(2):
                        ins = h.matmul(bk[bb0 + nh], lhsT=Xe[:, kk, :], rhs=wv[:, k8, nh * 512:(nh + 1) * 512],
                                       start=(kk == 0), stop=(kk == KD - 1))
                return ins
            k.op("pe", emit_gu, R=[r_ring[s], r_XeT[e % 2]], W=[rb[bb0], rb[bb0 + 1]])
            issue_slab()

        def down_slab(e, dh):
            eb = e % 2
            s = next_slab()
            wv = ring[s].rearrange("p (c n) -> p c n", c=KH)
            for nn in range(2):
                nb = 2 * dh + nn
                b = next_bank()

                def emit_d(h, nn=nn, b=b):
                    ins = None
                    for c in range(KH):
                        ins = h.matmul(bk[b], lhsT=hidT[:, c, :], rhs=wv[:, c, nn * 512:(nn + 1) * 512],
                                       start=(c == 0), stop=(c == KH - 1))
                    return ins
                k.op("pe", emit_d, R=[r_ring[s], r_hidT], W=[rb[b]])
                k.op("dve", lambda h, nb=nb, b=b: h.tensor_scalar(
                    out=Yb[eb][:, nb * 512:(nb + 1) * 512], in0=bk[b], scalar1=wslot[:, e:e + 1], scalar2=None,
                    op0=ALU.mult), R=[rb[b], r_ws], W=[r_Yb[eb]])
            issue_slab()

        prep(0)
        for q in range(4):
            gather(0, q)
        for e in range(n_exp):
            has_prev, has_next = e > 0, e + 1 < n_exp
            pair = e - 2 if (e >= 2 and e % 2 == 0) else None
            gu_slab(e, "g", 0)
            if pair is not None:
                combine_groups(pair, 0, 11)
            gu_slab(e, "g", 1)
            for nh in range(2):
                k.op("act", lambda h, nh=nh: h.activation(out=sg[nh][:, :], in_=bk[nh], func=AF.Silu),
                     R=[rb[nh]], W=[r_sg[nh]])
            if pair is not None:
                combine_groups(pair, 11, 22)
            gu_slab(e, "u", 0)
            if pair is not None:
                combine_groups(pair, 22, 32)
            if has_next:
                prep(e + 1)
            gu_slab(e, "u", 1)
            for nh in range(2):
                k.op("dve", lambda h, nh=nh: h.tensor_tensor(out=hid[:, nh * 512:(nh + 1) * 512], in0=bk[2 + nh],
                                                               in1=sg[nh][:, :], op=ALU.mult),
                     R=[rb[2 + nh], r_sg[nh]], W=[r_hid])

            def emit_hT(h):
                ins = None
                for c in range(KH):
                    ins = h.transpose(bkb[6][:, c * P:(c + 1) * P], hid[:, c * P:(c + 1) * P], ident_b[:, :])
                return ins
            k.op("pe", emit_hT, R=[r_hid, r_cb], W=[rb[6]])
            k.op("act", lambda h: h.copy(out=hidT, in_=bkb[6][:, :].rearrange("p (c s) -> p c s", c=KH)),
                 R=[rb[6]], W=[r_hidT])
            down_slab(e, 0)
            if has_next:
                gather(e + 1, 0)
                gather(e + 1, 1)
            down_slab(e, 1)
            if has_next:
                gather(e + 1, 2)
                gather(e + 1, 3)
        assert n_exp % 2 == 0
        combine_groups(n_exp - 2, 0, 32)

        obuf = [arena_t[:, ring_off // 4 + j * D:ring_off // 4 + (j + 1) * D] for j in range(4)]
        NOB = len(obuf)
        gb3 = arena_t[:, ring_off // 4 + 4 * D:ring_off // 4 + 5 * D]
        junk2 = arena_t[:, ring_off // 4 + 5 * D:ring_off // 4 + 5 * D + D // 2].bitcast(BF16)
        ring_users = [t for r in r_ring[0:3] for t in (list(r.r) + [r.w])]
        r_gb3 = Res()
        k.dma("sp", gb3, g3.partition_broadcast(P), gb_ds, W=[r_gb3], extra=ring_users)
        yb0_users = ring_users
        r_ob = [Res() for _ in range(NOB)]
        for i in range(NT):
            k.op("act", lambda h, i=i: h.activation(out=junk2[:, :], in_=xres[:, i, :], func=AF.Square,
                                                    accum_out=ss[:, 20 + i:21 + i]), R=[r_x[i + 1]], W=[r_ss], extra=yb0_users)
        k.op("act", lambda h: h.activation(out=rs[:, 20:28], in_=ss[:, 20:28], func=AF.Sqrt, scale=1.0 / D, bias=EPS),
             R=[r_ss], W=[r_rs])
        k.op("dve", lambda h: h.reciprocal(out=rstd[:, 20:28], in_=rs[:, 20:28]), R=[r_rs], W=[r_rstd])
        dso = [k.dsem("o%d" % s) for s in range(NOB)]
        otoks = []
        for i in range(NT):
            s = i % NOB
            k.op("dve", lambda h, i=i, s=s: h.scalar_tensor_tensor(
                out=obuf[s][:, :], in0=xres[:, i, :], scalar=rstd[:, 20 + i:21 + i], in1=gb3,
                op0=ALU.mult, op1=ALU.mult), R=[r_x[i + 1], r_rstd, r_gb3], W=[r_ob[s]],
                extra=ring_users)
            otoks.append(k.dma("sp", out[i * P:(i + 1) * P, :], obuf[s][:, :], dso[s], R=[r_ob[s]]))
        k._wait(k.engs["sp"], k.all_tokens())
    return nc


_CACHE = {}


def _consts():
    ident = np.eye(P, dtype=np.float32)
    iota = np.broadcast_to(np.arange(P, dtype=np.float32)[None, :], (P, P)).copy()
    tri = (np.arange(P)[:, None] < np.arange(P)[None, :]).astype(np.float32)
    ones = np.ones((P, P), dtype=np.float32)
    return ident, iota, tri, ones


def make_in_maps(x, norm_mix_g, w_in, w_pool, pool_scale, conv_w, w_out, norm_ffn_g,
                 w_router_group, b_router_group, w_router_expert, b_router_expert,
                 w_gate, w_up, w_down, norm_final_g):
    f = lambda a: np.ascontiguousarray(np.asarray(a, dtype=np.float32))
    x = f(x)[0]
    ident, iota, tri, ones = _consts()
    xpad = np.concatenate([np.zeros((HALO, D), np.float32), x], axis=0)
    pscale = f(f(pool_scale)[0].reshape(8, P).T)
    convw = f(f(conv_w)[0].reshape(8, P, 3).transpose(1, 0, 2).reshape(P, 24))
    w_r = f(np.concatenate([f(w_router_group)[0], f(w_router_expert)[0]], axis=1))
    b_r = f(np.broadcast_to(np.concatenate([f(b_router_group)[0], f(b_router_expert)[0]])[None, :], (P, 36)))
    shared = {
        "g1": f(norm_mix_g)[0], "g2": f(norm_ffn_g)[0], "g3": f(norm_final_g),
        "w_in": f(w_in)[0], "w_pool": f(w_pool)[0], "w_out": f(w_out)[0],
        "pscale": pscale, "convw": convw, "w_r": w_r, "b_r": b_r,
        "w_gate": f(w_gate)[0], "w_up": f(w_up)[0], "w_down": f(w_down)[0],
        "ident": ident, "iota": iota, "tri": tri, "ones": ones,
    }
    in_maps = []
    for c in range(NCORES):
        t0 = c * TOK
        xh = np.ascontiguousarray(xpad[t0:t0 + TX])
        pos = t0 + np.arange(HALO) + 1
        inv = np.stack([1.0 / np.minimum(pos, w) for w in (2, 4, 8, 16)]).astype(np.float32)
        invcnt = f(np.broadcast_to(inv.reshape(1, 64), (P, 64)))
        m = dict(shared)
        m["xh"] = xh
        m["invcnt"] = invcnt
        in_maps.append(m)
    return in_maps


def kernel(**inputs):
    in_maps = make_in_maps(**inputs)
    if "nc" not in _CACHE:
        _CACHE["nc"] = build_nc()
    res = run_bass_kernel_spmd(_CACHE["nc"], in_maps, core_ids=list(range(NCORES)))
    outs = [np.asarray(r["out"], dtype=np.float32) for r in res.results]
    return np.concatenate(outs, axis=0).reshape(1, NCORES * TOK, D)
```

```python
import numpy as np
from contextlib import ExitStack
import concourse.bass as bass
import concourse.mybir as mybir
from concourse.bass_utils import run_bass_kernel_spmd

F32 = mybir.dt.float32
BF16 = mybir.dt.bfloat16
AF = mybir.ActivationFunctionType
ALU = mybir.AluOpType
AX = mybir.AxisListType

P = 128
D = 2048
KD = 16
NT = 8
TOK = 1024
HALO = 16
TX = TOK + HALO
NE = 32
DE = 1024
KH = 8
NCORES = 8
EPS = 1e-6
BIG = 1.0e30
NRING = 4
NWIN = 3
RSHIFT = 1
MAXFLY = 8


class Tok:
    __slots__ = ("sem", "val", "key")

    def __init__(self, sem, val, key):
        self.sem, self.val, self.key = sem, val, key


class Res:
    __slots__ = ("w", "r", "const")

    def __init__(self, const=False):
        self.w = None
        self.r = []
        self.const = const


class DSem:
    def __init__(self, sem, key):
        self.sem, self.key, self.count = sem, key, 0


class Eng:
    def __init__(self, name, h, sem):
        self.name, self.h, self.sem = name, h, sem
        self.count = 0
        self.waited = {}


class Tracker:
    def __init__(self, nc, es):
        self.nc, self.es = nc, es
        self.engs = {}
        for name, h in (("pe", nc.tensor), ("act", nc.scalar), ("dve", nc.vector),
                        ("pool", nc.gpsimd), ("sp", nc.sync)):
            self.engs[name] = Eng(name, h, es.enter_context(nc.semaphore("e_" + name)))
        self.dsems = []

    def dsem(self, name):
        d = DSem(self.es.enter_context(self.nc.semaphore("d_" + name)), "d_" + name)
        self.dsems.append(d)
        return d

    def _wait(self, E, deps):
        need = {}
        for d in deps:
            if d is None:
                continue
            cur = need.get(d.key)
            if cur is None or cur.val < d.val:
                need[d.key] = d
        for key, d in need.items():
            if E.waited.get(key, 0) < d.val:
                E.h.wait_ge(d.sem, d.val)
                E.waited[key] = d.val

    @staticmethod
    def _deps(R, W, extra):
        deps = list(extra)
        for r in R:
            deps.append(r.w)
        for w in W:
            deps.append(w.w)
            deps.extend(w.r)
        return deps

    @staticmethod
    def _commit(tok, R, W):
        for r in R:
            if not r.const:
                r.r.append(tok)
        for w in W:
            w.w = tok
            w.r = []

    def op(self, eng, emit, R=(), W=(), extra=()):
        E = self.engs[eng]
        self._wait(E, self._deps(R, W, extra))
        ins = emit(E.h)
        E.count += 1
        ins.then_inc(E.sem, 1)
        tok = Tok(E.sem, E.count, E.name)
        self._commit(tok, R, W)
        return tok

    def dma(self, queue, out, in_, ds, R=(), W=(), extra=(), nodep=False):
        E = self.engs[queue]
        if not nodep:
            self._wait(E, self._deps(R, W, extra))
        E.h.dma_start(out=out, in_=in_).then_inc(ds.sem, 16)
        ds.count += 16
        tok = Tok(ds.sem, ds.count, ds.key)
        self._commit(tok, R, W)
        return tok

    def all_tokens(self):
        toks = []
        for E in self.engs.values():
            if E.count > 0:
                toks.append(Tok(E.sem, E.count, E.name))
        for d in self.dsems:
            if d.count > 0:
                toks.append(Tok(d.sem, d.count, d.key))
        return toks

    def barrier(self):
        toks = self.all_tokens()
        for E in self.engs.values():
            self._wait(E, toks)


class Arena:
    def __init__(self, t, nbytes):
        self.t, self.nbytes, self.off = t, nbytes, 0

    def alloc(self, nelem, dtype):
        size = 4 if dtype == F32 else 2
        nb = (nelem * size + 63) // 64 * 64
        off = self.off
        self.off += nb
        assert self.off <= self.nbytes, ("arena overflow", self.off, self.nbytes)
        ap = self.t[:, off // 4:(off + nb) // 4]
        if dtype != F32:
            ap = ap.bitcast(dtype)
        return ap[:, 0:nelem]


ARENA_BYTES = 212480


def build_nc(dbg=False, stop_after=None):
    nc = bass.Bass("TRN2", target_bir_lowering=False)
    dt = nc.dram_tensor
    xh = dt("xh", [TX, D], F32, kind="ExternalInput").ap()
    g1 = dt("g1", [D], F32, kind="ExternalInput").ap()
    g2 = dt("g2", [D], F32, kind="ExternalInput").ap()
    g3 = dt("g3", [D], F32, kind="ExternalInput").ap()
    w_in = dt("w_in", [D, 4096], F32, kind="ExternalInput").ap()
    w_pool = dt("w_pool", [4, 256, 256], F32, kind="ExternalInput").ap()
    w_out = dt("w_out", [D, D], F32, kind="ExternalInput").ap()
    pscale_d = dt("pscale", [P, 8], F32, kind="ExternalInput").ap()
    convw_d = dt("convw", [P, 24], F32, kind="ExternalInput").ap()
    invcnt_d = dt("invcnt", [P, 64], F32, kind="ExternalInput").ap()
    w_r = dt("w_r", [D, 36], F32, kind="ExternalInput").ap()
    b_r = dt("b_r", [P, 36], F32, kind="ExternalInput").ap()
    w_gate = dt("w_gate", [NE, D, DE], F32, kind="ExternalInput").ap()
    w_up = dt("w_up", [NE, D, DE], F32, kind="ExternalInput").ap()
    w_down = dt("w_down", [NE, DE, D], F32, kind="ExternalInput").ap()
    ident_d = dt("ident", [P, P], F32, kind="ExternalInput").ap()
    iota_d = dt("iota", [P, P], F32, kind="ExternalInput").ap()
    tri_d = dt("tri", [P, P], F32, kind="ExternalInput").ap()
    ones_d = dt("ones", [P, P], F32, kind="ExternalInput").ap()
    out = dt("out", [TOK, D], F32, kind="ExternalOutput").ap()
    if dbg:
        dbg_x1 = dt("dbg_x1", [TOK, D], F32, kind="ExternalOutput").ap()
        dbg_rt = dt("dbg_rt", [P, 3 * NT * NE], F32, kind="ExternalOutput").ap()

    with ExitStack() as es:
        arena_t = es.enter_context(nc.sbuf_tensor("arena", [P, ARENA_BYTES // 4], F32))
        banks = [es.enter_context(nc.psum_tensor("pb%d" % i, [P, 512], F32)) for i in range(8)]
        bk = [b[:, :] for b in banks]
        bkb = [b[:, :].bitcast(BF16) for b in banks]
        rb = [Res() for _ in range(8)]
        k = Tracker(nc, es)
        A = Arena(arena_t, ARENA_BYTES)

        xres = A.alloc(NT * D, F32).rearrange("p (i d) -> p i d", i=NT)
        gb = A.alloc(D, F32)
        hT_flat = A.alloc(KD * TX, BF16)
        hT = hT_flat.rearrange("p (k t) -> p k t", k=KD)
        h2 = hT_flat[:, 0:NT * D].rearrange("p (i d) -> p i d", i=NT)
        ident_f = A.alloc(P, F32)
        iota_f = A.alloc(P, F32)
        ident_b = A.alloc(P, BF16)
        tri_b = A.alloc(P, BF16)
        ones_b = A.alloc(P, BF16)
        wr_sb = A.alloc(KD * 36, F32).rearrange("p (k n) -> p k n", k=KD)
        br = A.alloc(36, F32)
        pscale = A.alloc(8, F32)
        convw = A.alloc(24, F32).rearrange("p (j c) -> p j c", j=8)
        invcnt = A.alloc(64, F32).rearrange("p (g t) -> p g t", g=4)
        ss = A.alloc(32, F32)
        rs = A.alloc(32, F32)
        rstd = A.alloc(32, F32)
        Afl = A.alloc(NT * NE, F32).rearrange("p (i e) -> p i e", i=NT)
        Abf = A.alloc(NT * NE, BF16).rearrange("p (i e) -> p i e", i=NT)
        Wb = A.alloc(NT * NE, BF16).rearrange("p (i e) -> p i e", i=NT)
        Wfl = A.alloc(NT * NE, F32).rearrange("p (i e) -> p i e", i=NT)
        rank = A.alloc(NT * NE, F32).rearrange("p (i e) -> p i e", i=NT)
        wslot = A.alloc(NE, F32)
        phase_mark = A.off

        r_cf = Res(const=True)
        r_cb = Res(const=True)
        r_gb = Res()
        r_x = [Res() for _ in range(NT + 1)]
        r_hT = Res()
        r_ss, r_rs, r_rstd = Res(), Res(), Res()

        cd_sp = k.dsem("c_sp")
        cd_pl = k.dsem("c_pl")
        k.dma("sp", gb, g1.partition_broadcast(P), k.dsem("gb"), W=[r_gb], nodep=True)
        gb_ds = k.dsems[-1]

        def load_consts_sp():
            for dst, src in ((ident_f, ident_d[:, :]), (iota_f, iota_d[:, :]), (br, b_r[:, :]),
                             (pscale, pscale_d[:, :]), (convw, convw_d.rearrange("p (j c) -> p j c", j=8)),
                             (invcnt, invcnt_d.rearrange("p (g t) -> p g t", g=4)),
                             (wr_sb, w_r.rearrange("(k p) n -> p k n", p=P))):
                t = k.dma("sp", dst, src, cd_sp, nodep=True)
            r_cf.w = t

        mixT = A.alloc(KD * TOK, BF16).rearrange("p (c t) -> p c t", c=KD)
        win = [A.alloc(KD * P, BF16).rearrange("p (k c) -> p k c", k=KD) for _ in range(NWIN)]
        fb = [A.alloc(TX, F32) for _ in range(4)]
        tA = A.alloc(TX, F32)
        tB = A.alloc(TX, F32)
        hn_off = A.off
        hn = [A.alloc(D, BF16) for _ in range(2)]
        pooledT = [A.alloc(2 * TOK, BF16).rearrange("p (k t) -> p k t", k=2) for _ in range(2)]
        wpool_b = A.alloc(4 * 2 * 256, BF16).rearrange("p (g k d) -> p g k d", g=4, k=2)
        c16 = A.alloc(16, F32)
        xhalo_flat = arena_t[:, (phase_mark + KD * TOK * 2 + NWIN * KD * P * 2) // 4:
                             (phase_mark + KD * TOK * 2 + NWIN * KD * P * 2) // 4 + D]
        xhalo = xhalo_flat

        for dst, src in ((ident_b, ident_d[:, :]), (tri_b, tri_d[:, :]), (ones_b, ones_d[:, :]),
                         (wpool_b, w_pool.rearrange("g (k p) d -> p g k d", p=P))):
            t = k.dma("pool", dst, src, cd_pl, nodep=True)
        r_cb.w = t

        dsx = [k.dsem("x%d" % i) for i in range(NT + 1)]
        xsrc = [None] * (NT + 1)
        npt = [HALO] + [P] * NT
        xtoks = []
        for ti in list(range(1, NT + 1)) + [0]:
            if len(xtoks) >= 2:
                k._wait(k.engs["sp"], [xtoks[-2]])
            if ti == 0:
                xsrc[ti] = xhalo[0:HALO, :]
                xtoks.append(k.dma("sp", xsrc[ti], xh[0:HALO, :], dsx[ti], W=[r_x[ti]], nodep=True))
            else:
                xsrc[ti] = xres[:, ti - 1, :]
                xtoks.append(k.dma("sp", xsrc[ti], xh[HALO + (ti - 1) * P:HALO + ti * P, :], dsx[ti], W=[r_x[ti]],
                                   nodep=True))
        load_consts_sp()

        k.op("dve", lambda h: h.memset(ss[:, :], 1.0), W=[r_ss])
        r_hn = [Res(), Res()]
        r_tA = Res()
        sqjunk = tA[:, 0:1024].bitcast(BF16)

        def norm_stats(ti):
            n = npt[ti]
            k.op("act", lambda h: h.activation(out=sqjunk[0:n, :], in_=xsrc[ti], func=AF.Square,
                                               accum_out=ss[0:n, ti:ti + 1]),
                 R=[r_x[ti]], W=[r_ss, r_tA])
            k.op("act", lambda h: h.activation(out=rs[:, ti:ti + 1], in_=ss[:, ti:ti + 1], func=AF.Sqrt,
                                               scale=1.0 / D, bias=EPS), R=[r_ss], W=[r_rs])
            k.op("dve", lambda h: h.reciprocal(out=rstd[:, ti:ti + 1], in_=rs[:, ti:ti + 1]), R=[r_rs], W=[r_rstd])

        seq = list(range(1, NT + 1)) + [0]
        pos_of = {ti: pos for pos, ti in enumerate(seq)}

        def tp_bank(ti, half):
            return (6 if pos_of[ti] % 2 == 0 else 4) + half

        def norm_apply(ti):
            n = npt[ti]
            par = pos_of[ti] % 2
            hb = hn[par]
            k.op("dve", lambda h: h.scalar_tensor_tensor(
                out=hb[0:n, :], in0=xsrc[ti], scalar=rstd[0:n, ti:ti + 1], in1=gb[0:n, :],
                op0=ALU.mult, op1=ALU.mult), R=[r_x[ti], r_rstd, r_gb], W=[r_hn[par]])
            for half in range(2):
                b = tp_bank(ti, half)

                def emit_tp(h, half=half, b=b):
                    ins = None
                    for j in range(8):
                        c = half * 8 + j
                        ins = h.transpose(bkb[b][:, j * n:(j + 1) * n], hb[0:n, c * P:(c + 1) * P], ident_b[0:n, 0:n])
                    return ins
                k.op("pe", emit_tp, R=[r_hn[par], r_cb], W=[rb[b]])

        def norm_evac(ti):
            n = npt[ti]
            toff = 0 if ti == 0 else HALO + (ti - 1) * P
            for half in range(2):
                b = tp_bank(ti, half)
                dst = hT[:, half * 8:half * 8 + 8, toff:toff + n]
                srcp = bkb[b][:, 0:8 * n].rearrange("p (k n) -> p k n", k=8)
                if half == 0:
                    k.op("act", lambda h, dst=dst, srcp=srcp: h.copy(out=dst, in_=srcp), R=[rb[b]], W=[r_hT])
                else:
                    k.op("dve", lambda h, dst=dst, srcp=srcp: h.tensor_copy(out=dst, in_=srcp), R=[rb[b]], W=[r_hT])

        for ti in seq[0:3]:
            norm_stats(ti)
        for pos, ti in enumerate(seq):
            norm_apply(ti)
            if pos >= 1:
                norm_evac(seq[pos - 1])
            if pos + 3 < len(seq):
                norm_stats(seq[pos + 3])
        norm_evac(seq[-1])
        k.dma("sp", gb, g2.partition_broadcast(P), gb_ds, W=[r_gb])

        order = list(range(8)) + [c for j in range(8) for c in (8 + j, 16 + j, 24 + j)]
        dswin = [k.dsem("win%d" % s) for s in range(NWIN)]
        r_win = [Res() for _ in range(NWIN)]
        r_fb = [Res() for _ in range(4)]
        r_tB, r_c16 = Res(), Res()
        r_pooled = [Res(), Res()]
        r_mix = [Res() for _ in range(KD)]
        pjsets = [(0, 1, 4), (2, 3, 5)]
        pjctr = [0]
        fbctr = [0]

        def win_dma(idx):
            cc = order[idx]
            s = idx % NWIN
            k.dma("pool", win[s], w_in[:, cc * P:(cc + 1) * P].rearrange("(k p) c -> p k c", p=P),
                  dswin[s], W=[r_win[s]])

        for idx in range(NWIN):
            win_dma(idx)

        dswo = [k.dsem("wo%d" % s) for s in range(2)]
        r_wo = [Res(), Res()]
        wout_sl = [arena_t[:, hn_off // 4:hn_off // 4 + 4096].bitcast(BF16).rearrange("p (k c) -> p k c", k=KD),
                   hT_flat[:, 0:8192].rearrange("p (k c) -> p k c", k=KD)]
        wout_extra = [[], []]

        def wout_dma(n):
            s = n % 2
            k.dma("pool", wout_sl[s], w_out[:, n * 512:(n + 1) * 512].rearrange("(k p) c -> p k c", p=P),
                  dswo[s], W=[r_wo[s]], extra=wout_extra[s])

        pending_pool = []

        def flush_pool():
            while pending_pool:
                gi, par = pending_pool.pop(0)
                for m in range(2):
                    pb0, pb1, _ = pjsets[pjctr[0] % 2]
                    pjctr[0] += 1

                    def emit_pm(h, gi=gi, m=m, par=par, pb0=pb0, pb1=pb1):
                        ins = None
                        for tb, pbb in ((0, pb0), (1, pb1)):
                            for kc2 in range(2):
                                ins = h.matmul(bk[pbb], lhsT=wpool_b[:, gi, kc2, m * P:(m + 1) * P],
                                               rhs=pooledT[par][:, kc2, tb * 512:(tb + 1) * 512],
                                               start=(kc2 == 0), stop=(kc2 == 1))
                        return ins
                    tpe = k.op("pe", emit_pm, R=[r_pooled[par], r_cb], W=[rb[pb0], rb[pb1]])
                    mc = 2 * gi + m

                    def emit_pe(h, mc=mc, pb0=pb0, pb1=pb1):
                        h.tensor_scalar(out=mixT[:, mc, 0:512], in0=bk[pb0], scalar1=pscale[:, mc:mc + 1],
                                        scalar2=None, op0=ALU.mult)
                        return h.tensor_scalar(out=mixT[:, mc, 512:1024], in0=bk[pb1], scalar1=pscale[:, mc:mc + 1],
                                               scalar2=None, op0=ALU.mult)
                    tdv = k.op("dve", emit_pe, R=[rb[pb0], rb[pb1], r_cf], W=[r_mix[mc]])
                if gi == 3:
                    wout_extra[0] = [tpe, tdv, r_cb.w]
                    wout_dma(0)

        conv_fb = {}
        for idx, cc in enumerate(order):
            s = idx % NWIN
            b0, b1, bh = pjsets[pjctr[0] % 2]
            pjctr[0] += 1

            def emit_proj(h, s=s, b0=b0, b1=b1, bh=bh):
                ins = None
                for kk in range(KD):
                    st, sp_ = (kk == 0), (kk == KD - 1)
                    h.matmul(bk[b0], lhsT=win[s][:, kk, :], rhs=hT[:, kk, HALO:HALO + 512], start=st, stop=sp_)
                    h.matmul(bk[b1], lhsT=win[s][:, kk, :], rhs=hT[:, kk, HALO + 512:TX], start=st, stop=sp_)
                    ins = h.matmul(bk[bh][:, 0:HALO], lhsT=win[s][:, kk, :], rhs=hT[:, kk, 0:HALO], start=st, stop=sp_)
                return ins
            k.op("pe", emit_proj, R=[r_win[s], r_hT], W=[rb[b0], rb[b1], rb[bh]])
            if idx + NWIN < len(order):
                win_dma(idx + NWIN)
            f = fbctr[0] % 4
            fbctr[0] += 1

            def emit_ev(h, f=f, b0=b0, b1=b1, bh=bh):
                h.copy(out=fb[f][:, HALO:HALO + 512], in_=bk[b0])
                h.copy(out=fb[f][:, HALO + 512:TX], in_=bk[b1])
                return h.copy(out=fb[f][:, 0:HALO], in_=bk[bh][:, 0:HALO])
            k.op("act", emit_ev, R=[rb[b0], rb[b1], rb[bh]], W=[r_fb[f]])
            flush_pool()

            if cc < 8:
                gi, kc = cc // 2, cc % 2
                w = 2 << gi
                par = gi % 2
                u = fb[f]
                cur, rcur, lo, shift = u, r_fb[f], 0, 1
                for l in range(gi + 1):
                    dst, rdst = (tA, r_tA) if l % 2 == 0 else (tB, r_tB)
                    lo2 = lo + shift
                    k.op("dve", lambda h, dst=dst, cur=cur, lo2=lo2, shift=shift: h.tensor_tensor(
                        out=dst[:, lo2:TX], in0=cur[:, lo2:TX], in1=cur[:, lo2 - shift:TX - shift], op=ALU.add),
                        R=[rcur], W=[rdst])
                    cur, rcur, lo, shift = dst, rdst, lo2, shift * 2
                k.op("dve", lambda h, cur=cur, u=u, par=par, kc=kc, w=w: h.scalar_tensor_tensor(
                    out=pooledT[par][:, kc, :], in0=cur[:, HALO:TX], scalar=1.0 / w, in1=u[:, HALO:TX],
                    op0=ALU.mult, op1=ALU.subtract), R=[rcur, r_fb[f]], W=[r_pooled[par]])
                k.op("dve", lambda h, cur=cur, gi=gi: h.tensor_tensor(
                    out=c16[:, :], in0=cur[:, HALO:2 * HALO], in1=invcnt[:, gi, :], op=ALU.mult),
                    R=[rcur, r_cf], W=[r_c16])
                k.op("dve", lambda h, u=u, par=par, kc=kc: h.tensor_tensor(
                    out=pooledT[par][:, kc, 0:HALO], in0=c16[:, :], in1=u[:, HALO:2 * HALO], op=ALU.subtract),
                    R=[r_c16, r_fb[f]], W=[r_pooled[par]])
                if kc == 1:
                    pending_pool.append((gi, par))
            else:
                j = (cc - 8) % 8
                conv_fb.setdefault(j, {})[(cc - 8) // 8] = f
                if cc >= 24:
                    fbb, fc, fv = conv_fb[j][0], conv_fb[j][1], conv_fb[j][2]
                    k.op("dve", lambda h, fc=fc, fv=fv: h.tensor_tensor(
                        out=tA[:, 14:TX], in0=fb[fc][:, 14:TX], in1=fb[fv][:, 14:TX], op=ALU.mult),
                        R=[r_fb[fc], r_fb[fv]], W=[r_tA])
                    k.op("dve", lambda h, j=j: h.tensor_scalar(
                        out=tB[:, HALO:TX], in0=tA[:, 14:TX - 2], scalar1=convw[:, j, 0:1], scalar2=None, op0=ALU.mult),
                        R=[r_tA, r_cf], W=[r_tB])
                    k.op("dve", lambda h, j=j: h.scalar_tensor_tensor(
                        out=tB[:, HALO:TX], in0=tA[:, 15:TX - 1], scalar=convw[:, j, 1:2], in1=tB[:, HALO:TX],
                        op0=ALU.mult, op1=ALU.add), R=[r_tA, r_tB, r_cf], W=[r_tB])
                    k.op("dve", lambda h, j=j: h.scalar_tensor_tensor(
                        out=tB[:, HALO:TX], in0=tA[:, HALO:TX], scalar=convw[:, j, 2:3], in1=tB[:, HALO:TX],
                        op0=ALU.mult, op1=ALU.add), R=[r_tA, r_tB, r_cf], W=[r_tB])
                    k.op("dve", lambda h, j=j, fbb=fbb: h.tensor_tensor(
                        out=mixT[:, 8 + j, :], in0=fb[fbb][:, HALO:TX], in1=tB[:, HALO:TX], op=ALU.mult),
                        R=[r_fb[fbb], r_tB], W=[r_mix[8 + j]])

        hT_users = list(r_hT.r) + [r_hT.w]
        wout_extra[1] = hT_users
        wout_dma(1)
        bctr = 0
        junk1c = fb[0][:, 0:1024].bitcast(BF16)
        for n in range(4):
            s = n % 2
            for i in range(NT):
                b = bctr % 4
                bctr += 1

                def emit_op(h, s=s, i=i, b=b):
                    ins = None
                    for kk in range(KD):
                        ins = h.matmul(bk[b], lhsT=mixT[:, kk, i * P:(i + 1) * P], rhs=wout_sl[s][:, kk, :],
                                       start=(kk == 0), stop=(kk == KD - 1))
                    return ins
                k.op("pe", emit_op, R=[r_wo[s]] + r_mix, W=[rb[b]])
                k.op("dve", lambda h, i=i, n=n, b=b: h.tensor_tensor(
                    out=xres[:, i, n * 512:(n + 1) * 512], in0=bk[b], in1=xres[:, i, n * 512:(n + 1) * 512], op=ALU.add),
                    R=[rb[b], r_x[i + 1]], W=[r_x[i + 1]])
                if n == 3:
                    k.op("act", lambda h, i=i: h.activation(out=junk1c, in_=xres[:, i, :], func=AF.Square,
                                                            accum_out=ss[:, 10 + i:11 + i]),
                         R=[r_x[i + 1]], W=[r_ss, r_fb[0]])
            if n + 2 < 4:
                wout_dma(n + 2)

        if dbg:
            dd = k.dsem("dbg")
            for i in range(NT):
                k.dma("sp", dbg_x1[i * P:(i + 1) * P, :], xres[:, i, :], dd, R=[r_x[i + 1]])

        k.barrier()
        if stop_after == "1C":
            return nc

        ring_off = ARENA_BYTES - NRING * 16384
        ring = [arena_t[:, (ring_off + s * 16384) // 4:(ring_off + (s + 1) * 16384) // 4].bitcast(BF16)
                for s in range(NRING)]
        dsr = [k.dsem("ring%d" % s) for s in range(NRING)]
        r_ring = [Res() for _ in range(NRING)]
        n_exp = NE if stop_after != "E1" else 4
        slabs = []
        for e in range(n_exp):
            for kind in ("g", "u"):
                slabs.append((kind, e, 0))
                slabs.append((kind, e, 1))
            slabs.append(("d", e, 0))
            slabs.append(("d", e, 1))
        slab_ptr = [0]
        slab_toks = []

        def issue_slab():
            if slab_ptr[0] >= len(slabs):
                return
            kind, e, hh = slabs[slab_ptr[0]]
            s = (slab_ptr[0] + RSHIFT) % NRING
            slab_ptr[0] += 1
            if kind == "d":
                dst = ring[s].rearrange("p (c n) -> p c n", c=KH)
                src = w_down[e, :, hh * 1024:(hh + 1) * 1024].rearrange("(c p) n -> p c n", p=P)
            else:
                wsrc = w_gate if kind == "g" else w_up
                dst = ring[s].rearrange("p (k n) -> p k n", k=8)
                src = wsrc[e].rearrange("(p k) n -> p k n", p=P)[:, hh * 8:(hh + 1) * 8, :]
            gate = [slab_toks[-MAXFLY]] if len(slab_toks) >= MAXFLY else []
            slab_toks.append(k.dma("pool", dst, src, dsr[s], W=[r_ring[s]], extra=gate))

        for _ in range(NRING - RSHIFT):
            issue_slab()

        A.off = phase_mark
        h2f_l = [A.alloc(D, F32) for _ in range(2)]
        h2fT_l = [A.alloc(KD * P, F32).rearrange("p (k t) -> p k t", k=KD) for _ in range(2)]
        lgT_sb = [A.alloc(P, F32) for _ in range(2)]
        r_lgT = [Res(), Res()]
        lg_all = A.alloc(NT * 36, F32).rearrange("p (i n) -> p i n", i=NT)
        r_h2f_l, r_h2fT_l = [Res(), Res()], [Res(), Res()]
        r_h2 = [Res() for _ in range(NT)]
        r_A, r_W, r_rank = Res(), Res(), Res()
        r_lg = Res()

        k.op("act", lambda h: h.activation(out=rs[:, 10:18], in_=ss[:, 10:18], func=AF.Sqrt, scale=1.0 / D, bias=EPS),
             R=[r_ss], W=[r_rs])
        k.op("dve", lambda h: h.reciprocal(out=rstd[:, 10:18], in_=rs[:, 10:18]), R=[r_rs], W=[r_rstd])

        def stage_a1(i):
            h2f, h2fT = h2f_l[i % 2], h2fT_l[i % 2]
            r_h2f, r_h2fT = r_h2f_l[i % 2], r_h2fT_l[i % 2]
            k.op("dve", lambda h: h.scalar_tensor_tensor(
                out=h2f[:, :], in0=xres[:, i, :], scalar=rstd[:, 10 + i:11 + i], in1=gb[:, :],
                op0=ALU.mult, op1=ALU.mult), R=[r_x[i + 1], r_rstd, r_gb], W=[r_h2f])
            k.op("pool", lambda h: h.tensor_copy(out=h2[:, i, :].rearrange("t (k p) -> t k p", p=P),
                                                 in_=h2f[:, :].rearrange("t (p k) -> t k p", k=KD)),
                 R=[r_h2f], W=[r_h2[i]])
            for q in range(4):
                def emit_tf(h, q=q):
                    ins = None
                    for j in range(4):
                        c = 4 * q + j
                        ins = h.transpose(bk[q][:, j * P:(j + 1) * P], h2f[:, c * P:(c + 1) * P], ident_f[:, :])
                    return ins
                k.op("pe", emit_tf, R=[r_h2f, r_cf], W=[rb[q]])
                ev_eng = "act" if q % 2 == 0 else "dve"
                if ev_eng == "act":
                    k.op("act", lambda h, q=q: h.copy(out=h2fT[:, 4 * q:4 * q + 4, :],
                                                      in_=bk[q].rearrange("p (k n) -> p k n", k=4)),
                         R=[rb[q]], W=[r_h2fT])
                else:
                    k.op("dve", lambda h, q=q: h.tensor_copy(out=h2fT[:, 4 * q:4 * q + 4, :],
                                                             in_=bk[q].rearrange("p (k n) -> p k n", k=4)),
                         R=[rb[q]], W=[r_h2fT])

        def stage_a2(i):
            h2fT = h2fT_l[i % 2]
            r_h2fT = r_h2fT_l[i % 2]
            lgb = 4 + 2 * (i % 2)

            def emit_lg(h):
                ins = None
                for kk in range(KD):
                    ins = h.matmul(bk[lgb][0:36, 0:P], lhsT=wr_sb[:, kk, :], rhs=h2fT[:, kk, :],
                                   start=(kk == 0), stop=(kk == KD - 1))
                return ins
            k.op("pe", emit_lg, R=[r_h2fT, r_cf], W=[rb[lgb]])
            k.op("act", lambda h: h.copy(out=lgT_sb[i % 2][0:36, :], in_=bk[lgb][0:36, 0:P]),
                 R=[rb[lgb]], W=[r_lgT[i % 2]])

        def stage_a3(i):
            tb = 5 + 2 * (i % 2)
            k.op("pe", lambda h: h.transpose(bk[tb][:, 0:36], lgT_sb[i % 2][0:36, :], ident_f[0:36, 0:36]),
                 R=[r_lgT[i % 2], r_cf], W=[rb[tb]])
            k.op("dve", lambda h: h.tensor_tensor(out=lg_all[:, i, :], in0=bk[tb][:, 0:36], in1=br[:, :], op=ALU.add),
                 R=[rb[tb], r_cf], W=[r_lg])

        stage_a1(0)
        for i in range(NT):
            if i + 1 < NT:
                stage_a1(i + 1)
            stage_a2(i)
            if i >= 1:
                stage_a3(i - 1)
        stage_a3(NT - 1)

        def al(n):
            return A.alloc(n, F32)
        gmax, gsum, gw = al(NT), al(NT), al(NT)
        gsh, gexp, gmask, pen = al(NT * 4), al(NT * 4), al(NT * 4), al(NT * 4)
        em, mask1, em2, mask2, w1m, w2m = (al(NT * NE) for _ in range(6))
        top1, top2, dlt, ed, den, p1, wt1, wt2 = (al(NT) for _ in range(8))
        v3 = lambda ap_, n: ap_.rearrange("p (i n) -> p i n", i=NT)
        bc = lambda ap_, n: ap_.unsqueeze(2).to_broadcast([P, NT, n])
        gl = lg_all[:, :, 0:4]
        el = lg_all[:, :, 4:36]
        rr = {n_: Res() for n_ in ("gmax", "gsh", "gexp", "gsum", "gw", "gmask", "pen", "em", "mask1", "em2", "mask2",
                                   "top1", "top2", "dlt", "ed", "den", "p1", "wt1", "wt2", "w1m", "w2m")}
        dv = lambda emit, R, W: k.op("dve", emit, R=R, W=W)
        dv(lambda h: h.reduce_max(out=gmax, in_=gl, axis=AX.X), [r_lg], [rr["gmax"]])
        dv(lambda h: h.tensor_tensor(out=v3(gsh, 4), in0=gl, in1=bc(gmax, 4), op=ALU.subtract), [r_lg, rr["gmax"]], [rr["gsh"]])
        k.op("act", lambda h: h.activation(out=gexp, in_=gsh, func=AF.Exp), R=[rr["gsh"]], W=[rr["gexp"]])
        dv(lambda h: h.reduce_sum(out=gsum, in_=v3(gexp, 4), axis=AX.X), [rr["gexp"]], [rr["gsum"]])
        dv(lambda h: h.reciprocal(out=gw, in_=gsum), [rr["gsum"]], [rr["gw"]])
        dv(lambda h: h.tensor_tensor(out=v3(gmask, 4), in0=gl, in1=bc(gmax, 4), op=ALU.is_equal), [r_lg, rr["gmax"]], [rr["gmask"]])
        dv(lambda h: h.tensor_scalar(out=pen, in0=gmask, scalar1=-1.0, scalar2=BIG, op0=ALU.add, op1=ALU.mult),
           [rr["gmask"]], [rr["pen"]])
        dv(lambda h: h.tensor_tensor(out=em.rearrange("p (i g j) -> p i g j", i=NT, g=4),
                                     in0=el.rearrange("p i (g j) -> p i g j", g=4),
                                     in1=v3(pen, 4).unsqueeze(3).to_broadcast([P, NT, 4, 8]), op=ALU.add),
           [r_lg, rr["pen"]], [rr["em"]])
        dv(lambda h: h.reduce_max(out=top1, in_=v3(em, NE), axis=AX.X), [rr["em"]], [rr["top1"]])
        def per_tile(out_, in_, sc, op):
            def emit(h):
                ins = None
                for i in range(NT):
                    ins = h.tensor_scalar(out=out_[:, i * NE:(i + 1) * NE], in0=in_[:, i * NE:(i + 1) * NE],
                                          scalar1=sc[:, i:i + 1], scalar2=None, op0=op)
                return ins
            return emit
        dv(per_tile(mask1, em, top1, ALU.is_equal), [rr["em"], rr["top1"]], [rr["mask1"]])
        dv(lambda h: h.scalar_tensor_tensor(out=em2, in0=mask1, scalar=-BIG, in1=em, op0=ALU.mult, op1=ALU.add),
           [rr["em"], rr["mask1"]], [rr["em2"]])
        dv(lambda h: h.reduce_max(out=top2, in_=v3(em2, NE), axis=AX.X), [rr["em2"]], [rr["top2"]])
        dv(per_tile(mask2, em2, top2, ALU.is_equal), [rr["em2"], rr["top2"]], [rr["mask2"]])
        dv(lambda h: h.tensor_tensor(out=dlt, in0=top2, in1=top1, op=ALU.subtract), [rr["top1"], rr["top2"]], [rr["dlt"]])
        k.op("act", lambda h: h.activation(out=ed, in_=dlt, func=AF.Exp), R=[rr["dlt"]], W=[rr["ed"]])
        dv(lambda h: h.tensor_scalar(out=den, in0=ed, scalar1=1.0, scalar2=None, op0=ALU.add), [rr["ed"]], [rr["den"]])
        dv(lambda h: h.reciprocal(out=p1, in_=den), [rr["den"]], [rr["p1"]])
        dv(lambda h: h.tensor_tensor(out=wt1, in0=p1, in1=gw, op=ALU.mult), [rr["p1"], rr["gw"]], [rr["wt1"]])
        dv(lambda h: h.tensor_tensor(out=wt2, in0=wt1, in1=ed, op=ALU.mult), [rr["wt1"], rr["ed"]], [rr["wt2"]])
        Afl2 = Afl.rearrange("p i e -> p (i e)")
        Wfl2 = Wfl.rearrange("p i e -> p (i e)")
        dv(lambda h: h.tensor_tensor(out=Afl2, in0=mask1, in1=mask2, op=ALU.add), [rr["mask1"], rr["mask2"]], [r_A])
        dv(lambda h: h.tensor_copy(out=Abf.rearrange("p i e -> p (i e)"), in_=Afl2), [r_A], [r_A])
        dv(per_tile(w1m, mask1, wt1, ALU.mult), [rr["mask1"], rr["wt1"]], [rr["w1m"]])
        dv(per_tile(w2m, mask2, wt2, ALU.mult), [rr["mask2"], rr["wt2"]], [rr["w2m"]])
        dv(lambda h: h.tensor_tensor(out=Wfl2, in0=w1m, in1=w2m, op=ALU.add), [rr["w1m"], rr["w2m"]], [r_W])
        dv(lambda h: h.tensor_copy(out=Wb.rearrange("p i e -> p (i e)"), in_=Wfl2), [r_W], [r_W])

        for i in range(NT):
            def emit_rk(h, i=i):
                ins = None
                for i2 in range(i):
                    h.matmul(bk[5][:, 0:NE], lhsT=ones_b[:, :], rhs=Abf[:, i2, :], start=(i2 == 0), stop=False)
                ins = h.matmul(bk[5][:, 0:NE], lhsT=tri_b[:, :], rhs=Abf[:, i, :], start=(i == 0), stop=True)
                return ins
            k.op("pe", emit_rk, R=[r_A, r_cb], W=[rb[5]])
            k.op("dve", lambda h, i=i: h.tensor_copy(out=rank[:, i, :], in_=bk[5][:, 0:NE]), R=[rb[5]], W=[r_rank])


        if dbg:
            dd2 = k.dsem("dbg2")
            k.dma("sp", dbg_rt[:, 0:NT * NE], Afl.rearrange("p i e -> p (i e)"), dd2, R=[r_A])
            k.dma("sp", dbg_rt[:, NT * NE:2 * NT * NE], Wfl.rearrange("p i e -> p (i e)"), dd2, R=[r_W])
            k.dma("sp", dbg_rt[:, 2 * NT * NE:3 * NT * NE], rank.rearrange("p i e -> p (i e)"), dd2, R=[r_rank])

        assert A.off <= ring_off + RSHIFT * 16384, ("router-phase scratch runs into prefetched ring slots", A.off, ring_off)
        k.barrier()

        A.off = phase_mark
        gb_bf = gb.bitcast(BF16)
        Yb0 = gb_bf[:, 0:D]
        ST = [gb_bf[:, D + j * TOK:D + (j + 1) * TOK] for j in range(2)]
        ob_off = A.off
        XeT1 = A.alloc(KD * P, BF16).rearrange("p (k s) -> p k s", k=KD)
        XeT = [XeT1, XeT1]
        S0 = A.alloc(NT * P, BF16).rearrange("p (i s) -> p i s", i=NT)
        S = [S0, S0]
        hid = A.alloc(DE, BF16)
        hidT = A.alloc(KH * P, BF16).rearrange("p (c s) -> p c s", c=KH)
        sg = [wr_sb.rearrange("p k n -> p (k n)")[:, 0:512], A.alloc(512, F32)]
        Yb = [Yb0, A.alloc(D, BF16)]
        ST.append(A.alloc(TOK, BF16))
        ob_end = A.off
        assert A.off <= ring_off, ("expert-phase buffers run into the weight ring", A.off, ring_off)
        r_Yb = [Res(), Res()]
        r_ST = [Res(), Res(), Res()]
        r_XeT1 = Res()
        r_XeT = [r_XeT1, r_XeT1]
        r_S0 = Res()
        r_S = [r_S0, r_S0]
        r_hid, r_hidT = Res(), Res()
        r_sg = [Res(), Res()]
        r_ws = Res()

        for _ in range(RSHIFT):
            issue_slab()
        use_ptr = [RSHIFT]

        def next_slab():
            s = use_ptr[0] % NRING
            use_ptr[0] += 1
            return s

        rot = [0]

        def next_bank():
            b = 4 + rot[0] % 2
            rot[0] += 1
            return b

        def combine_groups(e0, lo, hi):
            for gidx in range(lo, hi):
                i, nb = gidx // 4, gidx % 4
                b = next_bank()

                def emit_c(h, i=i, nb=nb, b=b):
                    h.matmul(bk[b], lhsT=ST[e0 % 3][:, i * P:(i + 1) * P], rhs=Yb[e0 % 2][:, nb * 512:(nb + 1) * 512],
                             start=True, stop=False)
                    return h.matmul(bk[b], lhsT=ST[(e0 + 1) % 3][:, i * P:(i + 1) * P],
                                    rhs=Yb[(e0 + 1) % 2][:, nb * 512:(nb + 1) * 512], start=False, stop=True)
                k.op("pe", emit_c, R=[r_ST[e0 % 3], r_ST[(e0 + 1) % 3], r_Yb[0], r_Yb[1]], W=[rb[b]])
                k.op("dve", lambda h, i=i, nb=nb, b=b: h.tensor_tensor(
                    out=xres[:, i, nb * 512:(nb + 1) * 512], in0=bk[b], in1=xres[:, i, nb * 512:(nb + 1) * 512],
                    op=ALU.add), R=[rb[b], r_x[i + 1]], W=[r_x[i + 1]])

        def prep(e):
            eb = e % 2
            Se = S[eb]

            def emit_S(h):
                ins = None
                for i in range(NT):
                    ins = h.tensor_scalar(out=Se[:, i, :], in0=iota_f[:, :], scalar1=rank[:, i, e:e + 1],
                                          scalar2=Afl[:, i, e:e + 1], op0=ALU.is_equal, op1=ALU.mult)
                return ins
            k.op("dve", emit_S, R=[r_rank, r_A, r_cf], W=[r_S[eb]])

            def emit_STt(h):
                ins = None
                for i in range(NT):
                    ins = h.transpose(bkb[6][:, i * P:(i + 1) * P], Se[:, i, :], ident_b[:, :])
                return ins
            k.op("pe", emit_STt, R=[r_S[eb], r_cb], W=[rb[6]])
            k.op("act", lambda h: h.copy(out=ST[e % 3][:, :], in_=bkb[6][:, :]), R=[rb[6]], W=[r_ST[e % 3]])

            def emit_ws(h):
                ins = None
                for i in range(NT):
                    ins = h.matmul(bk[7][:, 0:1], lhsT=Se[:, i, :], rhs=Wb[:, i, e:e + 1], start=(i == 0), stop=(i == NT - 1))
                return ins
            k.op("pe", emit_ws, R=[r_S[eb], r_W], W=[rb[7]])
            k.op("act", lambda h: h.copy(out=wslot[:, e:e + 1], in_=bk[7][:, 0:1]), R=[rb[7]], W=[r_ws])

        def gather(e, q):
            eb = e % 2
            Se, Xe = S[eb], XeT[eb]
            b = next_bank()

            def emit_g(h):
                ins = None
                for dk in range(4):
                    c = 4 * q + dk
                    for i in range(NT):
                        ins = h.matmul(bk[b][:, dk * P:(dk + 1) * P], lhsT=h2[:, i, c * P:(c + 1) * P], rhs=Se[:, i, :],
                                       start=(i == 0), stop=(i == NT - 1))
                return ins
            k.op("pe", emit_g, R=[r_S[eb]] + r_h2, W=[rb[b]])
            k.op("act", lambda h: h.copy(out=Xe[:, 4 * q:4 * q + 4, :], in_=bk[b].rearrange("p (k s) -> p k s", k=4)),
                 R=[rb[b]], W=[r_XeT[eb]])

        def gu_slab(e, kind, hh):
            Xe = XeT[e % 2]
            bb0 = 0 if kind == "g" else 2
            s = next_slab()
            wv = ring[s].rearrange("p (k n) -> p k n", k=8)

            def emit_gu(h):
                ins = None
                for k8 in range(8):
                    kk = hh * 8 + k8
                    for nh in range(2):
                        ins = h.matmul(bk[bb0 + nh], lhsT=Xe[:, kk, :], rhs=wv[:, k8, nh * 512:(nh + 1) * 512],
                                       start=(kk == 0), stop=(kk == KD - 1))
                return ins
            k.op("pe", emit_gu, R=[r_ring[s], r_XeT[e % 2]], W=[rb[bb0], rb[bb0 + 1]])
            issue_slab()

        def down_slab(e, dh):
            eb = e % 2
            s = next_slab()
            wv = ring[s].rearrange("p (c n) -> p c n", c=KH)
            for nn in range(2):
                nb = 2 * dh + nn
                b = next_bank()

                def emit_d(h, nn=nn, b=b):
                    ins = None
                    for c in range(KH):
                        ins = h.matmul(bk[b], lhsT=hidT[:, c, :], rhs=wv[:, c, nn * 512:(nn + 1) * 512],
                                       start=(c == 0), stop=(c == KH - 1))
                    return ins
                k.op("pe", emit_d, R=[r_ring[s], r_hidT], W=[rb[b]])
                k.op("dve", lambda h, nb=nb, b=b: h.tensor_scalar(
                    out=Yb[eb][:, nb * 512:(nb + 1) * 512], in0=bk[b], scalar1=wslot[:, e:e + 1], scalar2=None,
                    op0=ALU.mult), R=[rb[b], r_ws], W=[r_Yb[eb]])
            issue_slab()

        prep(0)
        for q in range(4):
            gather(0, q)
        for e in range(n_exp):
            has_prev, has_next = e > 0, e + 1 < n_exp
            pair = e - 2 if (e >= 2 and e % 2 == 0) else None
            gu_slab(e, "g", 0)
            if pair is not None:
                combine_groups(pair, 0, 11)
            gu_slab(e, "g", 1)
            for nh in range(2):
                k.op("act", lambda h, nh=nh: h.activation(out=sg[nh][:, :], in_=bk[nh], func=AF.Silu),
                     R=[rb[nh]], W=[r_sg[nh]])
            if pair is not None:
                combine_groups(pair, 11, 22)
            gu_slab(e, "u", 0)
            if pair is not None:
                combine_groups(pair, 22, 32)
            if has_next:
                prep(e + 1)
            gu_slab(e, "u", 1)
            for nh in range(2):
                k.op("dve", lambda h, nh=nh: h.tensor_tensor(out=hid[:, nh * 512:(nh + 1) * 512], in0=bk[2 + nh],
                                                               in1=sg[nh][:, :], op=ALU.mult),
                     R=[rb[2 + nh], r_sg[nh]], W=[r_hid])

            def emit_hT(h):
                ins = None
                for c in range(KH):
                    ins = h.transpose(bkb[6][:, c * P:(c + 1) * P], hid[:, c * P:(c + 1) * P], ident_b[:, :])
                return ins
            k.op("pe", emit_hT, R=[r_hid, r_cb], W=[rb[6]])
            k.op("act", lambda h: h.copy(out=hidT, in_=bkb[6][:, :].rearrange("p (c s) -> p c s", c=KH)),
                 R=[rb[6]], W=[r_hidT])
            down_slab(e, 0)
            if has_next:
                gather(e + 1, 0)
                gather(e + 1, 1)
            down_slab(e, 1)
            if has_next:
                gather(e + 1, 2)
                gather(e + 1, 3)
        assert n_exp % 2 == 0

        obuf = [arena_t[:, ring_off // 4 + j * D:ring_off // 4 + (j + 1) * D] for j in range(4)]
        NOB = len(obuf)
        gb3 = arena_t[:, ring_off // 4 + 4 * D:ring_off // 4 + 5 * D]
        junk2 = arena_t[:, ring_off // 4 + 5 * D:ring_off // 4 + 5 * D + D // 2].bitcast(BF16)
        ring_users = [t for r in r_ring[0:3] for t in (list(r.r) + [r.w])]
        r_gb3 = Res()
        k.dma("sp", gb3, g3.partition_broadcast(P), gb_ds, W=[r_gb3], extra=ring_users)
        r_ob = [Res() for _ in range(NOB)]
        r_ssf = [Res() for _ in range(NT)]
        r_rsf = [Res() for _ in range(NT)]
        r_rstdf = [Res() for _ in range(NT)]
        r_junk2 = Res()
        dso = [k.dsem("o%d" % s) for s in range(NOB)]
        NPOOL = 2

        def fin_stats(i):
            k.op("act", lambda h: h.activation(out=junk2[:, :], in_=xres[:, i, :], func=AF.Square,
                                               accum_out=ss[:, 20 + i:21 + i]),
                 R=[r_x[i + 1]], W=[r_ssf[i], r_junk2], extra=ring_users)
            k.op("act", lambda h: h.activation(out=rs[:, 20 + i:21 + i], in_=ss[:, 20 + i:21 + i], func=AF.Sqrt,
                                               scale=1.0 / D, bias=EPS), R=[r_ssf[i]], W=[r_rsf[i]])

        def fin_apply(i):
            s = i % NOB
            k.op("dve", lambda h: h.reciprocal(out=rstd[:, 20 + i:21 + i], in_=rs[:, 20 + i:21 + i]),
                 R=[r_rsf[i]], W=[r_rstdf[i]])
            if i < NPOOL:
                k.op("pool", lambda h: h.tensor_scalar(out=obuf[s][:, :], in0=xres[:, i, :],
                                                       scalar1=rstd[:, 20 + i:21 + i], scalar2=None, op0=ALU.mult),
                     R=[r_x[i + 1], r_rstdf[i]], W=[r_ob[s]], extra=ring_users)
                k.op("pool", lambda h: h.tensor_tensor(out=obuf[s][:, :], in0=obuf[s][:, :], in1=gb3, op=ALU.mult),
                     R=[r_gb3], W=[r_ob[s]])
            else:
                k.op("dve", lambda h: h.scalar_tensor_tensor(
                    out=obuf[s][:, :], in0=xres[:, i, :], scalar=rstd[:, 20 + i:21 + i], in1=gb3,
                    op0=ALU.mult, op1=ALU.mult), R=[r_x[i + 1], r_rstdf[i], r_gb3], W=[r_ob[s]],
                    extra=ring_users)
            k.dma("sp", out[i * P:(i + 1) * P, :], obuf[s][:, :], dso[s], R=[r_ob[s]])

        for i in range(NT):
            combine_groups(n_exp - 2, 4 * i, 4 * i + 4)
            fin_stats(i)
            if i >= 1:
                fin_apply(i - 1)
        fin_apply(NT - 1)
        k._wait(k.engs["sp"], k.all_tokens())
    return nc


_CACHE = {}


def _consts():
    ident = np.eye(P, dtype=np.float32)
    iota = np.broadcast_to(np.arange(P, dtype=np.float32)[None, :], (P, P)).copy()
    tri = (np.arange(P)[:, None] < np.arange(P)[None, :]).astype(np.float32)
    ones = np.ones((P, P), dtype=np.float32)
    return ident, iota, tri, ones


def make_in_maps(x, norm_mix_g, w_in, w_pool, pool_scale, conv_w, w_out, norm_ffn_g,
                 w_router_group, b_router_group, w_router_expert, b_router_expert,
                 w_gate, w_up, w_down, norm_final_g):
    f = lambda a: np.ascontiguousarray(np.asarray(a, dtype=np.float32))
    x = f(x)[0]
    ident, iota, tri, ones = _consts()
    xpad = np.concatenate([np.zeros((HALO, D), np.float32), x], axis=0)
    pscale = f(f(pool_scale)[0].reshape(8, P).T)
    convw = f(f(conv_w)[0].reshape(8, P, 3).transpose(1, 0, 2).reshape(P, 24))
    w_r = f(np.concatenate([f(w_router_group)[0], f(w_router_expert)[0]], axis=1))
    b_r = f(np.broadcast_to(np.concatenate([f(b_router_group)[0], f(b_router_expert)[0]])[None, :], (P, 36)))
    shared = {
        "g1": f(norm_mix_g)[0], "g2": f(norm_ffn_g)[0], "g3": f(norm_final_g),
        "w_in": f(w_in)[0], "w_pool": f(w_pool)[0], "w_out": f(w_out)[0],
        "pscale": pscale, "convw": convw, "w_r": w_r, "b_r": b_r,
        "w_gate": f(w_gate)[0], "w_up": f(w_up)[0], "w_down": f(w_down)[0],
        "ident": ident, "iota": iota, "tri": tri, "ones": ones,
    }
    in_maps = []
    for c in range(NCORES):
        t0 = c * TOK
        xh = np.ascontiguousarray(xpad[t0:t0 + TX])
        pos = t0 + np.arange(HALO) + 1
        inv = np.stack([1.0 / np.minimum(pos, w) for w in (2, 4, 8, 16)]).astype(np.float32)
        invcnt = f(np.broadcast_to(inv.reshape(1, 64), (P, 64)))
        m = dict(shared)
        m["xh"] = xh
        m["invcnt"] = invcnt
        in_maps.append(m)
    return in_maps


def kernel(**inputs):
    in_maps = make_in_maps(**inputs)
    if "nc" not in _CACHE:
        _CACHE["nc"] = build_nc()
    res = run_bass_kernel_spmd(_CACHE["nc"], in_maps, core_ids=list(range(NCORES)))
    outs = [np.asarray(r["out"], dtype=np.float32) for r in res.results]
    return np.concatenate(outs, axis=0).reshape(1, NCORES * TOK, D)
```
